# Optimizing a Trainium2 kernel written in Bass

```python
import math
import jax, jax.numpy as jnp
from jax import lax
import numpy as np

D_MODEL = 1024
BATCH = 4
SEQ = 4096
DEPTH = 1
DEC_BATCH = 8
DEC_SEQ = 64
PAST_LEN = 1024

CHUNK = 64
N_HEADS = 8
N_KV_HEADS = 2
HEAD_DIM = 128
ATT_W = N_HEADS * HEAD_DIM
ROT_DIM = HEAD_DIM // 4
ROPE_THETA = 500000.0
N_IDX_HEADS = 8
IDX_DIM = 64
IDX_ROT_DIM = IDX_DIM // 4
TOPK_MAX = 256
Q_BLOCK = 128
D_CONV = D_MODEL
CONV_WIDTH = 31
N_GROUPS = 4
EXPERTS_PER_GROUP = 4
N_EXPERTS = N_GROUPS * EXPERTS_PER_GROUP
D_EXPERT = 256
TOP_K_EXPERTS = 2
ALPHA = (2 * DEPTH) ** 0.25
BETA = (8 * DEPTH) ** -0.25
LN_EPS = 1e-5
NEG_INF = -1e30
ADA_SCALE = 0.25
SPLIT_SIZES = (ATT_W, N_KV_HEADS * HEAD_DIM, N_KV_HEADS * HEAD_DIM,
               N_IDX_HEADS * IDX_DIM, IDX_DIM, N_IDX_HEADS, 2 * D_CONV, 2 * D_MODEL)
N_IN = sum(SPLIT_SIZES)
SPLIT_POINTS = tuple(sum(SPLIT_SIZES[:i + 1]) for i in range(len(SPLIT_SIZES) - 1))

kernel_name = "dsa_conformer_hmoe_streaming_step"


def layer_norm(x, g, b):
    xf = x.astype(jnp.float32)
    mu = jnp.mean(xf, -1, keepdims=True)
    var = jnp.mean(jnp.square(xf - mu), -1, keepdims=True)
    return ((xf - mu) * lax.rsqrt(var + LN_EPS) * g.astype(jnp.float32) + b.astype(jnp.float32)).astype(x.dtype)


def rope_partial(x, pos, rot_dim):
    half = rot_dim // 2
    inv_freq = ROPE_THETA ** (-jnp.arange(half, dtype=jnp.float32) / half)
    ang = pos.astype(jnp.float32)[:, None] * inv_freq[None, :]
    cos = jnp.cos(ang)[None, :, None, :]
    sin = jnp.sin(ang)[None, :, None, :]
    xr = x[..., :rot_dim].astype(jnp.float32)
    x1, x2 = xr[..., :half], xr[..., half:]
    rot = jnp.concatenate([x1 * cos - x2 * sin, x2 * cos + x1 * sin], -1).astype(x.dtype)
    return jnp.concatenate([rot, x[..., rot_dim:]], -1)


def modulation(c, w_ada, b_ada):
    m = jax.nn.silu(c) @ w_ada + b_ada
    return [t[:, None, :] for t in jnp.split(m, 6, axis=-1)]


def mixer_inputs(h, pos, w_in):
    B, T, _ = h.shape
    z = h @ w_in
    q, k, v, qi, ki, wi, glu, gl = jnp.split(z, SPLIT_POINTS, axis=-1)
    q = rope_partial(q.reshape(B, T, N_HEADS, HEAD_DIM), pos, ROT_DIM)
    k = rope_partial(k.reshape(B, T, N_KV_HEADS, HEAD_DIM), pos, ROT_DIM)
    v = v.reshape(B, T, N_KV_HEADS, HEAD_DIM)
    qi = rope_partial(qi.reshape(B, T, N_IDX_HEADS, IDX_DIM), pos, IDX_ROT_DIM)
    ki = rope_partial(ki.reshape(B, T, 1, IDX_DIM), pos, IDX_ROT_DIM)[:, :, 0]
    a, b = jnp.split(glu, 2, axis=-1)
    u = a * jax.nn.sigmoid(b)
    return q, k, v, qi, ki, wi, u, gl


def dsa_block(q, qi, wi, q_pos, k_all, v_all, ki_all, top_k):
    B, Q = q.shape[0], q.shape[1]
    f32 = jnp.float32
    dots = jnp.einsum('bqhd,bld->bqhl', qi, ki_all, preferred_element_type=f32) * (IDX_DIM ** -0.5)
    score = jnp.einsum('bqh,bqhl->bql', wi.astype(f32) * (N_IDX_HEADS ** -0.5), jax.nn.relu(dots))
    q_chunk = q_pos // CHUNK
    k_chunk = jnp.arange(k_all.shape[1], dtype=jnp.int32) // CHUNK
    admissible = k_chunk[None, :] <= q_chunk[:, None]
    score = jnp.where(admissible[None], score, NEG_INF)
    _, idx = lax.top_k(score, top_k)
    valid = (idx // CHUNK) <= q_chunk[None, :, None]
    gather = jax.vmap(lambda rows, ids: rows[ids])
    k_sel = gather(k_all, idx)
    v_sel = gather(v_all, idx)
    qg = q.reshape(B, Q, N_KV_HEADS, N_HEADS // N_KV_HEADS, HEAD_DIM)
    logits = jnp.einsum('bqgrd,bqkgd->bqgrk', qg, k_sel, preferred_element_type=f32) * (HEAD_DIM ** -0.5)
    logits = jnp.where(valid[:, :, None, None, :], logits, NEG_INF)
    p = jax.nn.softmax(logits, axis=-1).astype(v_all.dtype)
    o = jnp.einsum('bqgrk,bqkgd->bqgrd', p, v_sel)
    return o.reshape(B, Q, ATT_W)


def conv_branch(u_ext, w_dw, b_dw, g, b, w_out):
    y = lax.conv_general_dilated(u_ext, w_dw[:, None, :], (1,), 'VALID',
                                 dimension_numbers=('NWC', 'WIO', 'NWC'),
                                 feature_group_count=u_ext.shape[-1]) + b_dw
    y = jax.nn.silu(layer_norm(y, g, b))
    return y @ w_out


def hier_moe(h, w_group, b_group, w_router, b_router, w_gate_up, w_down):
    Bsz, T, D = h.shape
    hf = h.reshape(-1, D)
    n = hf.shape[0]
    gl = (hf @ w_group + b_group).astype(jnp.float32)
    g_idx = jnp.argmax(gl, axis=-1)
    g_w = jnp.max(jax.nn.softmax(gl, axis=-1), axis=-1)
    rl = (hf @ w_router + b_router).astype(jnp.float32).reshape(n, N_GROUPS, EXPERTS_PER_GROUP)
    sel_idx = jnp.broadcast_to(g_idx[:, None, None], (n, 1, EXPERTS_PER_GROUP))
    in_group = jnp.take_along_axis(rl, sel_idx, axis=1)[:, 0]
    top_val, top_idx = lax.top_k(in_group, TOP_K_EXPERTS)
    w_top = jax.nn.softmax(top_val, axis=-1) * g_w[:, None]
    expert_id = g_idx[:, None] * EXPERTS_PER_GROUP + top_idx
    combine = jnp.einsum('nk,nke->ne', w_top, jax.nn.one_hot(expert_id, N_EXPERTS, dtype=jnp.float32)).astype(h.dtype)
    gu = jnp.einsum('nd,edf->nef', hf, w_gate_up)
    gate, up = jnp.split(gu, 2, axis=-1)
    act = jax.nn.silu(gate) * up * combine[:, :, None]
    y = jnp.einsum('nef,efd->nd', act, w_down)
    return y.reshape(Bsz, T, D)


def layer_step(x, c, pos, past_k, past_v, past_ki, conv_state,
               w_ada, b_ada, w_in, w_att_out, w_dw, b_dw, conv_ln_g, conv_ln_b,
               w_conv_out, w_o, ln1_g, ln1_b, w_group, b_group, w_router, b_router,
               w_gate_up, w_down, ln2_g, ln2_b):
    B, T, _ = x.shape
    L = past_k.shape[1] + T
    top_k = min(TOPK_MAX, L // 4)
    sh1, sc1, g1, sh2, sc2, g2 = modulation(c, w_ada, b_ada)
    h = x * (1 + sc1) + sh1
    q, k, v, qi, ki, wi, u, gl = mixer_inputs(h, pos, w_in)
    k_all = jnp.concatenate([past_k, k], axis=1)
    v_all = jnp.concatenate([past_v, v], axis=1)
    ki_all = jnp.concatenate([past_ki, ki], axis=1)
    blk = min(T, Q_BLOCK)
    nb = T // blk
    to_blocks = lambda t: t.reshape((B, nb, blk) + t.shape[2:]).swapaxes(0, 1)
    per_block = lambda a: dsa_block(a[0], a[1], a[2], a[3], k_all, v_all, ki_all, top_k)
    o = lax.map(per_block, (to_blocks(q), to_blocks(qi), to_blocks(wi), pos.reshape(nb, blk)))
    o = o.swapaxes(0, 1).reshape(B, T, ATT_W)
    u_ext = jnp.concatenate([conv_state, u], axis=1)
    y_conv = conv_branch(u_ext, w_dw, b_dw, conv_ln_g, conv_ln_b, w_conv_out)
    g_att, g_conv = jnp.split(jax.nn.sigmoid(gl), 2, axis=-1)
    mix = (g_att * (o @ w_att_out) + g_conv * y_conv) @ w_o
    x = layer_norm(ALPHA * x + (1 + g1) * mix, ln1_g, ln1_b)
    h2 = x * (1 + sc2) + sh2
    x = layer_norm(ALPHA * x + (1 + g2) * hier_moe(h2, w_group, b_group, w_router, b_router, w_gate_up, w_down), ln2_g, ln2_b)
    return x, k, v, ki, u_ext[:, -(CONV_WIDTH - 1):]


def setup_inputs(seed: int = 0) -> dict:
    key = jax.random.key(seed)
    ks = jax.random.split(key, 32)
    nrm = lambda k, shape, s: jax.random.normal(k, shape, jnp.float32) * s
    D = D_MODEL
    return {
        "x_prompt": nrm(ks[0], (BATCH, SEQ, D), 1.0),
        "x_sample": nrm(ks[1], (DEC_BATCH, DEC_SEQ, D), 1.0),
        "cache_k": nrm(ks[2], (DEPTH, DEC_BATCH, PAST_LEN, N_KV_HEADS, HEAD_DIM), 1.0),
        "cache_v": nrm(ks[3], (DEPTH, DEC_BATCH, PAST_LEN, N_KV_HEADS, HEAD_DIM), 1.0),
        "cache_idx_k": nrm(ks[4], (DEPTH, DEC_BATCH, PAST_LEN, IDX_DIM), 1.0),
        "state_conv": nrm(ks[5], (DEPTH, DEC_BATCH, CONV_WIDTH - 1, D_CONV), 0.5),
        "c_prompt": nrm(ks[6], (BATCH, D), 1.0),
        "c_sample": nrm(ks[7], (DEC_BATCH, D), 1.0),
        "w_ada": nrm(ks[8], (DEPTH, D, 6 * D), ADA_SCALE * D ** -0.5),
        "b_ada": nrm(ks[9], (DEPTH, 6 * D), 0.01),
        "w_in": nrm(ks[10], (DEPTH, D, N_IN), D ** -0.5),
        "w_att_out": nrm(ks[11], (DEPTH, ATT_W, D), ATT_W ** -0.5),
        "w_dw": nrm(ks[12], (DEPTH, CONV_WIDTH, D_CONV), CONV_WIDTH ** -0.5),
        "b_dw": nrm(ks[13], (DEPTH, D_CONV), 0.01),
        "conv_ln_g": 1.0 + nrm(ks[14], (DEPTH, D_CONV), 0.01),
        "conv_ln_b": nrm(ks[15], (DEPTH, D_CONV), 0.01),
        "w_conv_out": nrm(ks[16], (DEPTH, D_CONV, D), D_CONV ** -0.5),
        "w_o": nrm(ks[17], (DEPTH, D, D), BETA * D ** -0.5),
        "ln1_g": 1.0 + nrm(ks[18], (DEPTH, D), 0.01),
        "ln1_b": nrm(ks[19], (DEPTH, D), 0.01),
        "w_group": nrm(ks[20], (DEPTH, D, N_GROUPS), D ** -0.5),
        "b_group": nrm(ks[21], (DEPTH, N_GROUPS), 0.01),
        "w_router": nrm(ks[22], (DEPTH, D, N_EXPERTS), D ** -0.5),
        "b_router": nrm(ks[23], (DEPTH, N_EXPERTS), 0.01),
        "w_gate_up": nrm(ks[24], (DEPTH, N_EXPERTS, D, 2 * D_EXPERT), D ** -0.5),
        "w_down": nrm(ks[25], (DEPTH, N_EXPERTS, D_EXPERT, D), BETA * D_EXPERT ** -0.5),
        "ln2_g": 1.0 + nrm(ks[26], (DEPTH, D), 0.01),
        "ln2_b": nrm(ks[27], (DEPTH, D), 0.01),
    }


def reference(x_prompt, x_sample, cache_k, cache_v, cache_idx_k, state_conv, c_prompt, c_sample,
              w_ada, b_ada, w_in, w_att_out, w_dw, b_dw, conv_ln_g, conv_ln_b, w_conv_out, w_o,
              ln1_g, ln1_b, w_group, b_group, w_router, b_router, w_gate_up, w_down, ln2_g, ln2_b):
    bp, tp = x_prompt.shape[0], x_prompt.shape[1]
    ts = x_sample.shape[1]
    past = cache_k.shape[2]
    pos_p = jnp.arange(tp, dtype=jnp.int32)
    pos_s = past + jnp.arange(ts, dtype=jnp.int32)
    yp, ys = x_prompt, x_sample
    kp_l, vp_l, kip_l, cp_l, ks_l, vs_l, kis_l, cs_l = [], [], [], [], [], [], [], []
    for layer in range(DEPTH):
        params = (w_ada[layer], b_ada[layer], w_in[layer], w_att_out[layer], w_dw[layer], b_dw[layer],
                  conv_ln_g[layer], conv_ln_b[layer], w_conv_out[layer], w_o[layer], ln1_g[layer], ln1_b[layer],
                  w_group[layer], b_group[layer], w_router[layer], b_router[layer], w_gate_up[layer],
                  w_down[layer], ln2_g[layer], ln2_b[layer])
        empty_kv = jnp.zeros((bp, 0, N_KV_HEADS, HEAD_DIM), cache_k.dtype)
        empty_ki = jnp.zeros((bp, 0, IDX_DIM), cache_idx_k.dtype)
        zero_conv = jnp.zeros((bp, CONV_WIDTH - 1, D_CONV), state_conv.dtype)
        yp, kp, vp, kip, cp = layer_step(yp, c_prompt, pos_p, empty_kv, empty_kv, empty_ki, zero_conv, *params)
        ys, kss, vss, kis, cs = layer_step(ys, c_sample, pos_s, cache_k[layer], cache_v[layer],
                                           cache_idx_k[layer], state_conv[layer], *params)
        kp_l.append(kp); vp_l.append(vp); kip_l.append(kip); cp_l.append(cp)
        ks_l.append(kss); vs_l.append(vss); kis_l.append(kis); cs_l.append(cs)
    return (yp, ys, jnp.stack(kp_l), jnp.stack(vp_l), jnp.stack(kip_l), jnp.stack(cp_l),
            jnp.stack(ks_l), jnp.stack(vs_l), jnp.stack(kis_l), jnp.stack(cs_l))
```

```python
import math
from contextlib import ExitStack

import numpy as np
import concourse.bass as bass
import concourse.mybir as mybir
from concourse.bass_utils import run_bass_kernel_spmd

F32 = mybir.dt.float32
F32R = mybir.dt.float32r
BF16 = mybir.dt.bfloat16
AF = mybir.ActivationFunctionType
ALU = mybir.AluOpType
AX = mybir.AxisListType

D = 1024
SEQ = 4096
PAST = 1024
TS = 64
NKC = 8
N_IN = 6216
ALPHA = 2.0 ** 0.25
LN_EPS = 1e-5
NEG = -1.0e30
MASKV = -30000.0
NIT = 13
TOPK = 256
ROPE_THETA = 500000.0
C_Q, C_K, C_V, C_QI, C_KI, C_WI, C_GA, C_GB, C_GATT, C_GCONV = 0, 1024, 1280, 1536, 2048, 2112, 2120, 3144, 4168, 5192
ISCALE = (64 ** -0.5) * (8 ** -0.5)
ASCALE = 128 ** -0.5

ENGS = ("pe", "act", "dve", "pool", "sp")


class Res:
    __slots__ = ("name", "w", "r")

    def __init__(self, name):
        self.name = name
        self.w = None
        self.r = {}


class Tok:
    __slots__ = ("eng", "idx", "sem", "val")

    def __init__(self, eng, idx, sem=None, val=None):
        self.eng = eng
        self.idx = idx
        self.sem = sem
        self.val = val


class Op:
    __slots__ = ("fn", "waits", "inc", "dma_sem")

    def __init__(self, fn):
        self.fn = fn
        self.waits = []
        self.inc = False
        self.dma_sem = None


class Sched:
    def __init__(self, n_dma_sems=8):
        self.ops = {e: [] for e in ENGS}
        self.inc_ord = {e: [] for e in ENGS}
        self.cnt = {e: 0 for e in ENGS}
        self.waited = {e: {} for e in ENGS}
        self.n_dma = n_dma_sems
        self.dma_k = {e: 0 for e in ENGS}
        self.dma_last = {}

    def _resolve(self, tok):
        if tok.sem is not None:
            return tok.sem, tok.val
        e = tok.eng
        if tok.val is not None:
            return ("eng", e), tok.val
        ords = self.inc_ord[e]
        i = tok.idx
        n = len(ords)
        j = i
        while j < n and ords[j] is None:
            j += 1
        if j < n:
            tok.val = ords[j]
        else:
            last = n - 1
            while last > i and (self.ops[e][last].dma_sem is not None or self.ops[e][last].fn is None):
                last -= 1
            self.cnt[e] += 1
            ords[last] = self.cnt[e]
            self.ops[e][last].inc = True
            tok.val = self.cnt[e]
        return ("eng", e), tok.val

    def _add_wait(self, eng, op, tok):
        if tok is None:
            return
        if tok.eng == "pe" and eng == "pe":
            return
        key, val = self._resolve(tok)
        w = self.waited[eng]
        if w.get(key, 0) >= val:
            return
        w[key] = val
        op.waits.append((key, val))

    def _deps(self, eng, op, reads, writes):
        for r in reads:
            self._add_wait(eng, op, r.w)
        for wr in writes:
            self._add_wait(eng, op, wr.w)
            for t in wr.r.values():
                self._add_wait(eng, op, t)

    def add(self, eng, fn, reads=(), writes=()):
        reads = [b.r if hasattr(b, "r") and isinstance(b.r, Res) else b for b in reads]
        writes = [b.r if hasattr(b, "r") and isinstance(b.r, Res) else b for b in writes]
        op = Op(fn)
        self._deps(eng, op, reads, writes)
        idx = len(self.ops[eng])
        self.ops[eng].append(op)
        self.inc_ord[eng].append(None)
        tok = Tok(eng, idx)
        for r in reads:
            r.r[eng] = tok
        for wr in writes:
            wr.w = tok
            wr.r = {}
        return tok

    def dma(self, eng, fn, reads=(), writes=()):
        reads = [b.r if hasattr(b, "r") and isinstance(b.r, Res) else b for b in reads]
        writes = [b.r if hasattr(b, "r") and isinstance(b.r, Res) else b for b in writes]
        op = Op(fn)
        k = self.dma_k[eng]
        self.dma_k[eng] = k + 1
        slot = k % self.n_dma
        key = ("dma", eng, slot)
        prev = self.dma_last.get(key, 0)
        if prev > 0:
            w = self.waited[eng]
            if w.get(key, 0) < 16 * prev:
                w[key] = 16 * prev
                op.waits.append((key, 16 * prev))
        self.dma_last[key] = prev + 1
        self._deps(eng, op, reads, writes)
        op.dma_sem = key
        idx = len(self.ops[eng])
        self.ops[eng].append(op)
        self.inc_ord[eng].append(None)
        tok = Tok(None, idx, sem=key, val=16 * (prev + 1))
        for r in reads:
            r.r[("dma", eng, slot, prev)] = tok
        for wr in writes:
            wr.w = tok
            wr.r = {}
        return tok

    def final_wait(self, eng, toks):
        op = Op(None)
        for t in toks:
            self._add_wait(eng, op, t)
        self.ops[eng].append(op)
        self.inc_ord[eng].append(None)

    def emit(self, nc, stack):
        sems = {}

        def sem(key):
            if key not in sems:
                sems[key] = stack.enter_context(nc.semaphore("s_" + "_".join(str(x) for x in key)))
            return sems[key]

        for e in ENGS:
            if self.cnt[e] > 0:
                sem(("eng", e))
        for key in sorted(self.dma_last.keys(), key=str):
            sem(key)
        block = stack.enter_context(nc.Block())

        def run(engname):
            def body(eng):
                for op in self.ops[engname]:
                    for key, val in op.waits:
                        eng.wait_ge(sem(key), val)
                    if op.fn is None:
                        continue
                    inst = op.fn(eng)
                    if op.dma_sem is not None:
                        inst.then_inc(sem(op.dma_sem), 16)
                    elif op.inc:
                        inst.then_inc(sem(("eng", engname)), 1)
            return body

        block.tensor(run("pe"))
        block.scalar(run("act"))
        block.vector(run("dve"))
        block.gpsimd(run("pool"))
        block.sync(run("sp"))


class B:
    def __init__(self, t, name):
        self.t = t
        self.r = Res(name)


def build_program(stage=99):
    nc = bass.Bass("TRN2", target_bir_lowering=False)
    nc.dge_precook = False
    S = Sched()
    st = ExitStack()

    def din(name, shape, dt=F32):
        return nc.dram_tensor(name, list(shape), dt, kind="ExternalInput").ap()

    def dout(name, shape):
        return nc.dram_tensor(name, list(shape), F32, kind="ExternalOutput").ap()

    xa = din("xa", [SEQ, D]); xo = din("xo", [2048, D]); xh = din("xh", [128, D]); hv = din("hv", [128, 4])
    xs = din("xs", [TS, D]); ck = din("ck", [PAST, 256]); cv = din("cv", [PAST, 256]); cki = din("cki", [PAST, 64])
    scvT = din("scvT", [D, 32], F32R); ccT = din("ccT", [128, 8, 2])
    w_ada = din("w_ada", [D, 6144], F32R); b_adaT = din("b_adaT", [128, 48])
    w_in = din("w_in", [D, N_IN], F32R); w_kv = din("w_kv", [D, 576], F32R); w_wi = din("w_wi", [D, 8], F32R)
    w_att = din("w_att", [D, D], F32R); w_co = din("w_co", [D, D], F32R); w_o = din("w_o", [D, D], F32R)
    wdwT = din("wdwT", [128, 8, 31]); cvec = din("cvec", [128, 3, 8])
    lnrow = din("lnrow", [4, D])
    w_rt = din("w_rt", [D, 20]); b_rt = din("b_rt", [1, 20])
    w_gu = din("w_gu", [16, D, 512], F32R); w_dn = din("w_dn", [16, 256, D], F32R)
    rope_all = din("rope_all", [SEQ, 96]); rope_own = din("rope_own", [2048, 96]); rope_s = din("rope_s", [TS, 96])
    amask = din("amask", [4, 128, 640]); ident_d = din("ident", [128, 128]); pw2 = din("pw2", [128, NIT])
    yo = dout("yo", [2048, D]); kvo = dout("kvo", [SEQ, 576]); cvo = dout("cvo", [32, D])
    ys = dout("ys", [TS, D]); kvs = dout("kvs", [TS, 576]); cvs = dout("cvs", [32, D])
    gscr = nc.dram_tensor("gscr", [32, 128], F32, kind="Internal").ap()

    def sb(name, shape, dt=F32):
        return st.enter_context(nc.sbuf_tensor(name, list(shape), dt))

    WS = [B(sb("WS%d" % i, [128, 8, 512], F32R), "WS%d" % i) for i in range(3)]
    HT = B(sb("HT", [128, 8, 544], F32R), "HT")
    HTR = {(g, p): Res("HT_%d_%d" % (g, p)) for g in range(5) for p in range(2)}

    def HTres(g):
        return [HTR[(g, 0)], HTR[(g, 1)]]

    HT_TOK = [HTR[(g, p)] for g in range(1, 5) for p in range(2)]
    A3 = B(sb("A3", [128, 8, 512], F32R), "A3")
    OX = B(sb("OX", [128, 8, 512], F32R), "OX")
    ysq = B(sb("ysq", [128, 512], F32R), "ysq")
    identr = B(sb("identr", [128, 128], F32R), "identr")
    onesr = B(sb("onesr", [128, 128], F32R), "onesr")
    siluT = B(sb("siluT", [128, 8, 2], F32R), "siluT")
    wwi = B(sb("wwi", [128, 8, 8], F32R), "wwi")
    KT = B(sb("KT", [128, 2, SEQ], BF16), "KT")
    VV = B(sb("VV", [128, 32, 256], BF16), "VV")
    KIT = B(sb("KIT", [128, SEQ], BF16), "KIT")
    A1 = B(sb("A1", [128, 4096], F32), "A1")
    A2 = B(sb("A2", [128, 4096], F32), "A2")
    A4 = B(sb("A4", [128, 3072], F32), "A4")
    XT = [B(sb("XT%d" % i, [128, D], F32), "XT%d" % i) for i in range(2)]
    kvsb = [B(sb("kvsb%d" % i, [128, 576], F32), "kvsb%d" % i) for i in range(2)]
    ident = B(sb("identf", [128, 128], F32), "identf")
    identb = B(sb("identb", [128, 128], BF16), "identb")
    onesb = B(sb("onesb", [128, 128], BF16), "onesb")
    modT = B(sb("modT", [128, 48, 2], F32), "modT")
    scp = B(sb("scp", [128, 2, 2, 8], F32), "scp")
    gpT = B(sb("gpT", [128, 32], F32), "gpT")
    badaT = B(sb("badaT", [128, 48], F32), "badaT")
    hvt = B(sb("hvt", [128, 4], F32), "hvt")
    wdw = B(sb("wdw", [128, 8, 31], F32), "wdw")
    cvc = B(sb("cvc", [128, 3, 8], F32), "cvc")
    wrt = B(sb("wrt", [128, 8, 20], F32), "wrt")
    brt = B(sb("brt", [128, 20], F32), "brt")
    amk = B(sb("amk", [128, 640], BF16), "amk")
    pw = B(sb("pw", [128, NIT], F32), "pw")
    rp = [B(sb("rp%d" % i, [128, 96], F32), "rp%d" % i) for i in range(2)]
    kb16 = B(amk.t[:, 0:384], "kb16")
    wsa = B(sb("wsa", [128, 4, 8], F32), "wsa")
    sm = B(sb("sm", [128, 32], F32), "sm")
    stp = B(sb("stp", [128, 3, NIT], F32), "stp")
    st6 = B(sb("st6", [128, 2, 6], F32), "st6")
    mv = B(sb("mv", [128, 2], F32), "mv")
    rt = B(sb("rt", [128, 96], F32), "rt")
    comb = B(sb("comb", [128, 4, 16], F32), "comb")
    gsb = B(A1.t[0:32, 128:256], "gsb"); gsb.r = A1.r
    ubr = B(sb("ubr", [128, 544], F32R), "ubr")
    ubf = ubr.t[:].bitcast(F32)
    dgc = [B(sb("dgc%d" % i, [128, 128], F32R), "dgc%d" % i) for i in range(4)]
    tA = B(A1.t[:, 3072:3328].rearrange("p (h d) -> p h d", h=8), "tA"); tA.r = A1.r
    tB = B(A1.t[:, 3328:3584].rearrange("p (h d) -> p h d", h=8), "tB"); tB.r = A1.r
    A3f = A3.t[:].rearrange("p a b -> p (a b)").bitcast(F32)
    A3r = A3.t[:].rearrange("p a b -> p (a b)")
    RR = [B(A3r[:, i * 512:(i + 1) * 512], "RR%d" % i) for i in range(4)]
    DG = B(A3r[:, 2048:3072].rearrange("p (h q) -> p h q", h=8), "DG")
    ACTT = B(A3r[:, 0:1024].rearrange("p (a b) -> p a b", a=2), "ACTT")
    _k0b = kvsb[0].t[:, 0:512].bitcast(BF16)
    PT = [B(_k0b[:, i * 512:(i + 1) * 512], "PT%d" % i) for i in range(2)]
    rec = B(kvsb[1].t[:, 0:512], "rec")
    A4b = A4.t[:].bitcast(BF16)
    QT = B(A4b[:, 0:4096].rearrange("p (h t) -> p h t", h=8), "QT")
    QST = B(A4b[:, 4096:6144].rearrange("p (a t) -> p a t", a=4), "QST")
    QSTH = B(XT[1].t[:].bitcast(BF16).rearrange("p (a t) -> p a t", a=4), "QSTH")
    ub = B(A4.t[:, 0:544], "ub")
    sg = B(A4.t[:, 544:1088], "sg")
    acc = B(A4.t[:, 1088:1600], "acc")
    A2b = A2.t[:].bitcast(BF16)
    MT = B(A2b[:, 0:4096].rearrange("p (k q) -> p k q", q=128), "MT")
    MQ = B(A2b[:, 4096:8192], "MQ")
    qb16 = B(A2b[:, 6144:8192], "qb16")
    lnt = B(A2.t[:, 0:1536].rearrange("p (a b) -> p a b", a=3), "lnt")
    uo = B(A2.t[0:32, 2048:3072], "uo")
    junk = B(XT[0].t[:].bitcast(mybir.dt.uint8), "junk"); junk.r = XT[0].r
    sgu = B(A1.t[:, 3072:3584], "sgu")

    def handoff(olds, news):
        for n_ in news:
            for o_ in olds:
                toks = ([o_.r.w] if o_.r.w is not None else []) + list(o_.r.r.values())
                for k_, t_ in enumerate(toks):
                    n_.r.r[("h", id(o_), k_)] = t_

    pst = st.enter_context(nc.psum_tensor("ps", [128, 7, 512], F32))
    psbf = st.enter_context(nc.psum_tensor("psbf", [128, 1024], BF16))
    PS = [B(None, "PS%d" % i) for i in range(8)]

    def ps(b, lo=0, hi=512):
        return pst[:, b, lo:hi]

    def psb(b):
        assert b == 7
        return psbf

    def A(eng, fn, reads=(), writes=()):
        return S.add(eng, fn, reads, writes)

    wcount = [0]

    def wload(src_ap, rows_kc=8, cols=512):
        slot = WS[wcount[0] % 3]
        wcount[0] += 1
        S.dma("sp", lambda e: e.dma_start(out=slot.t[:, 0:rows_kc, 0:cols],
                                          in_=src_ap.rearrange("(c p) n -> p c n", p=128)), writes=[slot])
        return slot

    out_toks = []

    S.dma("sp", lambda e: e.dma_start(out=ident.t[:], in_=ident_d), writes=[ident])
    A("dve", lambda e: e.tensor_copy(identb.t[:], ident.t[:]), [ident], [identb])
    A("act", lambda e: e.activation(out=identr.t[:], in_=ident.t[:], func=AF.Copy), [ident], [identr])
    A("dve", lambda e: e.memset(onesb.t[:], 1.0), [], [onesb])
    A("dve", lambda e: e.memset(A1.t[:, 0:128], 1.0), [], [A1])
    A("act", lambda e: e.activation(out=onesr.t[:], in_=A1.t[:, 0:128], func=AF.Copy), [A1], [onesr])
    for (dst, src) in [(badaT, b_adaT), (hvt, hv), (wdw, wdwT), (cvc, cvec), (pw, pw2)]:
        S.dma("sp", lambda e, dst=dst, src=src: e.dma_start(out=dst.t[:], in_=src), writes=[dst])
    S.dma("sp", lambda e: e.dma_start(out=wwi.t[:], in_=w_wi.rearrange("(c p) n -> p c n", p=128)), writes=[wwi])
    S.dma("sp", lambda e: e.dma_start(out=wrt.t[:], in_=w_rt.rearrange("(c p) n -> p c n", p=128)), writes=[wrt])
    S.dma("sp", lambda e: e.dma_start(out=brt.t[:], in_=b_rt.to_broadcast([128, 20])), writes=[brt])
    S.dma("sp", lambda e: e.dma_start(out=A2.t[:, 0:16].rearrange("p (a b) -> p a b", a=8), in_=ccT), writes=[A2])
    A("act", lambda e: e.activation(out=siluT.t[:], in_=A2.t[:, 0:16].rearrange("p (a b) -> p a b", a=8), func=AF.Silu), [A2], [siluT])

    for blk in range(12):
        slot = wload(w_ada[:, blk * 512:(blk + 1) * 512])
        for jj in range(4):
            j = blk * 4 + jj
            for kc in range(8):
                A("pe", lambda e, slot=slot, jj=jj, kc=kc, j=j: e.matmul(
                    pst[:, 0, 2 * j:2 * j + 2], slot.t[:, kc, jj * 128:(jj + 1) * 128], siluT.t[:, kc, :],
                    start=(kc == 0), stop=(kc == 7)), [slot, siluT], [PS[0]])
    A("dve", lambda e: e.tensor_tensor(out=modT.t[:], in0=pst[:, 0, 0:96].rearrange("p (j s) -> p j s", s=2),
                                       in1=badaT.t[:].unsqueeze(2).to_broadcast([128, 48, 2]), op=ALU.add),
      [PS[0], badaT], [modT])
    for v, base in enumerate((8, 32)):
        A("dve", lambda e, v=v, base=base: e.tensor_scalar(
            out=scp.t[:, v, :, :], in0=modT.t[:, base:base + 8, :].rearrange("p k s -> p s k"),
            scalar1=1.0, scalar2=None, op0=ALU.add), [modT], [scp])
    for v, base in enumerate((16, 40)):
        A("dve", lambda e, v=v, base=base: e.tensor_scalar(
            out=gpT.t[:, v * 16:(v + 1) * 16].rearrange("p (s k) -> p s k", s=2),
            in0=modT.t[:, base:base + 8, :].rearrange("p k s -> p s k"),
            scalar1=1.0, scalar2=None, op0=ALU.add), [modT], [gpT])
    A("pe", lambda e: e.transpose(pst[0:32, 1, 0:128], gpT.t[:], ident.t[:]), [gpT, ident], [PS[1]])
    A("dve", lambda e: e.tensor_copy(gsb.t[:], pst[0:32, 1, 0:128]), [PS[1]], [gsb])
    gs_res = Res("gscr")
    S.dma("pool", lambda e: e.dma_start(out=gscr, in_=gsb.t[:]), reads=[gsb], writes=[gs_res])
    gview = gscr.rearrange("(a k) p -> a (k p)", k=8)

    def finish():
        S.final_wait("pool", out_toks)
        S.emit(nc, st)
        st.close()
        return nc

    if stage == 0:
        out_toks.append(S.dma("pool", lambda e: e.dma_start(out=cvo[:, 0:128], in_=gsb.t[:]), reads=[gsb]))
        return finish()

    def sh_col(v, s, kc):
        base = 0 if v == 0 else 24
        return modT.t[:, base + kc, s:s + 1]

    def sc_col(v, s, kc):
        return scp.t[:, v, s, kc:kc + 1]

    xcnt = [0]

    def load_x(src_rows_ap, nrows=128):
        xb = XT[xcnt[0] % 2]
        xcnt[0] += 1
        S.dma("sp", lambda e: e.dma_start(out=xb.t[0:nrows, :], in_=src_rows_ap), writes=[xb])
        return xb

    tcnt = [0]

    def transpose_mod(xb, nrows, dst, dcol, v, s, src_t=None, g=None):
        pb = 2 + 2 * (tcnt[0] % 2)
        tcnt[0] += 1
        srct = xb.t if src_t is None else src_t
        for kc in range(8):
            b = pb + kc // 4
            A("pe", lambda e, kc=kc, b=b: e.transpose(pst[:, b, (kc % 4) * 128:(kc % 4) * 128 + nrows],
                                                      srct[0:nrows, kc * 128:(kc + 1) * 128], ident.t[0:nrows, 0:nrows]),
              [xb, ident], [PS[b]])
        for kc in range(8):
            b = pb + kc // 4
            src = pst[:, b, (kc % 4) * 128:(kc % 4) * 128 + nrows]
            if kc < 4:
                A("dve", lambda e, kc=kc, src=src: e.tensor_scalar(
                    out=dst.t[:, kc, dcol:dcol + nrows], in0=src, scalar1=sc_col(v, s, kc), scalar2=sh_col(v, s, kc),
                    op0=ALU.mult, op1=ALU.add), [PS[b], scp, modT], [HTR[(g, 0)]])
            else:
                A("act", lambda e, kc=kc, src=src: e.activation(
                    out=dst.t[:, kc, dcol:dcol + nrows], in_=src, func=AF.Identity,
                    scale=sc_col(v, s, kc), bias=sh_col(v, s, kc)), [PS[b], scp, modT], [HTR[(g, 1)]])

    def rope(buf, col0, nheads, hstride, half, tab, tcol, nrows):
        rot = 2 * half
        xv = buf.t[0:nrows, col0:col0 + nheads * hstride].rearrange("p (h d) -> p h d", h=nheads)
        cc = tab.t[0:nrows, tcol:tcol + rot].unsqueeze(1).to_broadcast([nrows, nheads, rot])
        sn1 = tab.t[0:nrows, tcol + rot:tcol + rot + half].unsqueeze(1).to_broadcast([nrows, nheads, half])
        sn2 = tab.t[0:nrows, tcol + rot + half:tcol + 2 * rot].unsqueeze(1).to_broadcast([nrows, nheads, half])
        a = tA.t[0:nrows, 0:nheads, 0:rot]
        b_ = tB.t[0:nrows, 0:nheads, 0:rot]
        A("dve", lambda e: e.tensor_tensor(out=a, in0=xv[:, :, 0:rot], in1=cc, op=ALU.mult), [buf, tab], [tA])
        A("dve", lambda e: e.tensor_tensor(out=b_[:, :, 0:half], in0=xv[:, :, half:rot], in1=sn1, op=ALU.mult), [buf, tab], [tB])
        A("dve", lambda e: e.tensor_tensor(out=b_[:, :, half:rot], in0=xv[:, :, 0:half], in1=sn2, op=ALU.mult), [buf, tab], [tB])
        A("dve", lambda e: e.tensor_tensor(out=xv[:, :, 0:rot], in0=a, in1=b_, op=ALU.add), [tA, tB], [buf])

    kvcnt = [0]

    import os
    SUB = int(os.environ.get("SUB", "9"))
    S.dma("sp", lambda e: e.dma_start(out=WS[0].t[:, :, 0:512], in_=w_kv[:, 0:512].rearrange("(c p) n -> p c n", p=128)), writes=[WS[0]])
    S.dma("sp", lambda e: e.dma_start(out=WS[1].t[:, :, 0:64], in_=w_kv[:, 512:576].rearrange("(c p) n -> p c n", p=128)), writes=[WS[1]])

    def kv_store(kvb, nrows, key_tile, krow0=0):
        A("act", lambda e: e.activation(out=VV.t[krow0:krow0 + nrows, key_tile, :], in_=kvb.t[0:nrows, 256:512], func=AF.Copy), [kvb], [VV])
        A("pool", lambda e: e.tensor_copy(kb16.t[0:nrows, 0:256], kvb.t[0:nrows, 0:256]), [kvb], [kb16])
        A("pool", lambda e: e.tensor_copy(kb16.t[0:nrows, 256:384].rearrange("p (s d) -> p s d", s=2),
                                          kvb.t[0:nrows, 512:576].unsqueeze(1).to_broadcast([nrows, 2, 64])), [kvb], [kb16])
        pv = psbf
        for g in range(2):
            A("pe", lambda e, g=g: e.transpose(pv[:, 256 + g * 128:256 + g * 128 + nrows], kb16.t[0:nrows, g * 128:(g + 1) * 128],
                                               identb.t[0:nrows, 0:nrows]), [kb16, identb], [PS[7]])
        A("pe", lambda e: e.transpose(pv[:, 512:512 + nrows], kb16.t[0:nrows, 256:384], identb.t[0:nrows, 0:nrows]),
          [kb16, identb], [PS[7]])
        k0 = key_tile * 128 + krow0
        A("act", lambda e: e.activation(out=KT.t[:, :, k0:k0 + nrows], in_=pv[:, 256:512].rearrange("p (g t) -> p g t", g=2)[:, :, 0:nrows],
                                        func=AF.Copy), [PS[7]], [KT])
        A("act", lambda e: e.activation(out=KIT.t[:, k0:k0 + nrows], in_=pv[:, 512:512 + nrows], func=AF.Copy), [PS[7]], [KIT])

    def kv_tile2(hT, hcol, nrows, rope_src_ap, out_ap, key_tile, store=True, g=1):
        i = kvcnt[0] % 2
        kvcnt[0] += 1
        kvb = kvsb[i]
        rpb = rp[i]
        pb = 0
        S.dma("sp", lambda e: e.dma_start(out=rpb.t[0:nrows, :], in_=rope_src_ap), writes=[rpb])
        for kc in range(8):
            A("pe", lambda e, kc=kc: e.matmul(pst[0:nrows, pb, 0:512], hT.t[:, kc, hcol:hcol + nrows], WS[0].t[:, kc, 0:512],
                                              start=(kc == 0), stop=(kc == 7)), HTres(g) + [WS[0]], [PS[pb]])
            A("pe", lambda e, kc=kc: e.matmul(pst[0:nrows, pb + 1, 0:64], hT.t[:, kc, hcol:hcol + nrows], WS[1].t[:, kc, 0:64],
                                              start=(kc == 0), stop=(kc == 7)), HTres(g) + [WS[1]], [PS[pb + 1]])
        A("act", lambda e: e.activation(out=kvb.t[0:nrows, 0:512], in_=pst[0:nrows, pb, 0:512], func=AF.Copy), [PS[pb]], [kvb])
        A("dve", lambda e: e.tensor_copy(kvb.t[0:nrows, 512:576], pst[0:nrows, pb + 1, 0:64]), [PS[pb + 1]], [kvb])
        rope(kvb, 0, 2, 128, 16, rpb, 0, nrows)
        rope(kvb, 512, 1, 64, 8, rpb, 64, nrows)
        out_toks.append(S.dma("pool", lambda e: e.dma_start(out=out_ap, in_=kvb.t[0:nrows, :]), reads=[kvb]))
        if store:
            kv_store(kvb, nrows, key_tile)
        return kvb

    def pp_stage1(n):
        xb = load_x(xa[n * 128:(n + 1) * 128, :])
        transpose_mod(xb, 128, HT, 32 + (n % 4) * 128, 0, 0, g=1 + n % 4)

    pp_kvb = {}
    pp_stage1(0)
    for n in range(32):
        if n + 1 < 32:
            pp_stage1(n + 1)
        pp_kvb[n] = kv_tile2(HT, 32 + (n % 4) * 128, 128, rope_all[n * 128:(n + 1) * 128, :], kvo[n * 128:(n + 1) * 128, :], n, store=False, g=1 + n % 4)
        if n >= 1:
            kv_store(pp_kvb[n - 1], 128, n - 1)
    kv_store(pp_kvb[31], 128, 31)

    if stage == 1:
        return finish()

    OXf = OX.t[:].rearrange("p a b -> p (a b)").bitcast(F32)
    OXr = OX.t[:].rearrange("p a b -> p (a b)")
    X1r = OXr.rearrange("p (t d) -> p t d", t=4)
    X1f = OXf.rearrange("p (t d) -> p t d", t=4)
    ZQ = [B(A1.t[:, 0:1536], "zq0"), B(A1.t[:, 1536:3072], "zq1"), B(A2.t[:, 0:1536], "zq2"), B(A2.t[:, 1536:3072], "zq3")]
    ZQ[0].r = A1.r; ZQ[1].r = A1.r; ZQ[2].r = A2.r; ZQ[3].r = A2.r
    SCR = B(A1.t, "SCR"); SCR.r = A1.r
    SG = B(A2.t[:, :].rearrange("p (c t) -> p c t", c=8), "SG"); SG.r = A2.r
    YA = B(A2.t[:, :].rearrange("p (t d) -> p t d", t=4), "YA"); YA.r = A2.r
    BC = [B(A1.t[:, i * 1024:(i + 1) * 1024], "BC%d" % i) for i in range(3)]
    BIG = 1.0e30

    def bn_ln(src_ap, nr, name):
        for h in range(2):
            A("dve", lambda e, h=h: e.bn_stats(out=st6.t[0:nr, h, :], in_=src_ap[:, h * 512:(h + 1) * 512]), [name], [st6])
        A("dve", lambda e: e.bn_aggr(out=mv.t[0:nr, :], in_=st6.t[0:nr].rearrange("p a b -> p (a b)")), [st6], [mv])
        A("act", lambda e: e.activation(out=sm.t[0:nr, 0:1], in_=mv.t[0:nr, 1:2], func=AF.Sqrt, bias=LN_EPS, scale=1.0), [mv], [sm])
        A("dve", lambda e: e.reciprocal(out=sm.t[0:nr, 0:1], in_=sm.t[0:nr, 0:1]), [sm], [sm])

    def make_hT_for(tiles, xsrc, halo_src, seq):
        if halo_src is not None:
            xb_ = load_x(halo_src, 32)
            transpose_mod(xb_, 32, HT, 0, 0, seq, g=0)
        for tt, (r0, nr) in enumerate(tiles):
            xb_ = load_x(xsrc[r0:r0 + nr, :], nr)
            transpose_mod(xb_, nr, HT, 32 + tt * 128, 0, seq, g=1 + tt)

    def process_st(ntok, tiles, xsrc, halo_src, halo_col, rope_src, seq, key_sizes_fn, use_amask, y_out, conv_out, conv_tok0,
                   hT_done=False, prefetch=None):
        nt = len(tiles)
        T0 = 32

        def make_hT():
            if halo_src is not None:
                xb_ = load_x(halo_src, 32)
                transpose_mod(xb_, 32, HT, 0, 0, seq, g=0)
            for tt, (r0, nr) in enumerate(tiles):
                xb_ = load_x(xsrc[r0:r0 + nr, :], nr)
                transpose_mod(xb_, nr, HT, T0 + tt * 128, 0, seq, g=1 + tt)

        if not hT_done:
            make_hT()
        handoff(kvsb, PT + [rec])
        for sl, c0 in enumerate((C_Q, C_Q + 512, C_QI)):
            slot = wload(w_in[:, c0:c0 + 512])
            for tt, (r0, nr) in enumerate(tiles):
                bank = (sl * nt + tt) % 2
                for kc in range(8):
                    A("pe", lambda e, kc=kc, bank=bank, tt=tt, nr=nr, slot=slot: e.matmul(
                        pst[0:nr, bank, :], HT.t[:, kc, T0 + tt * 128:T0 + tt * 128 + nr], slot.t[:, kc, :],
                        start=(kc == 0), stop=(kc == 7)), HTres(1 + tt) + [slot], [PS[bank]])
                if (sl + tt) % 2 == 0:
                    A("act", lambda e, bank=bank, tt=tt, nr=nr, sl=sl: e.activation(
                        out=ZQ[tt].t[0:nr, sl * 512:(sl + 1) * 512], in_=pst[0:nr, bank, :], func=AF.Copy), [PS[bank]], [ZQ[tt]])
                else:
                    A("dve", lambda e, bank=bank, tt=tt, nr=nr, sl=sl: e.tensor_copy(
                        ZQ[tt].t[0:nr, sl * 512:(sl + 1) * 512], pst[0:nr, bank, :]), [PS[bank]], [ZQ[tt]])
        for tt, (r0, nr) in enumerate(tiles):
            for kc in range(8):
                A("pe", lambda e, kc=kc, tt=tt, nr=nr: e.matmul(
                    pst[0:nr, 6, 0:8], HT.t[:, kc, T0 + tt * 128:T0 + tt * 128 + nr], wwi.t[:, kc, :],
                    start=(kc == 0), stop=(kc == 7)), HTres(1 + tt) + [wwi], [PS[6]])
            A("dve", lambda e, tt=tt, nr=nr: e.tensor_scalar(out=wsa.t[0:nr, tt, :], in0=pst[0:nr, 6, 0:8], scalar1=0.5 * ISCALE, scalar2=None,
                                                             op0=ALU.mult), [PS[6]], [wsa])
        for tt, (r0, nr) in enumerate(tiles):
            rpb = rp[tt % 2]
            S.dma("sp", lambda e, rpb=rpb, r0=r0, nr=nr: e.dma_start(out=rpb.t[0:nr, :], in_=rope_src[r0:r0 + nr, :]), writes=[rpb])
            rope(ZQ[tt], 0, 8, 128, 16, rpb, 0, nr)
            rope(ZQ[tt], 1024, 8, 64, 8, rpb, 64, nr)
            A("dve", lambda e, tt=tt, nr=nr: e.tensor_copy(qb16.t[0:nr, 0:1024], ZQ[tt].t[0:nr, 0:1024]), [ZQ[tt]], [qb16])
            A("dve", lambda e, tt=tt, nr=nr: e.tensor_copy(
                qb16.t[0:nr, 1024:2048].rearrange("p (h s d) -> p h s d", h=8, s=2),
                ZQ[tt].t[0:nr, 1024:1536].rearrange("p (h d) -> p h d", h=8).unsqueeze(2).to_broadcast([nr, 8, 2, 64])), [ZQ[tt]], [qb16])
            for h in range(8):
                A("pe", lambda e, h=h, nr=nr: e.transpose(psbf[:, h * 128:h * 128 + nr], qb16.t[0:nr, h * 128:(h + 1) * 128],
                                                          identb.t[0:nr, 0:nr]), [qb16, identb], [PS[7]])
            A("dve", lambda e, tt=tt, nr=nr: e.tensor_copy(
                QT.t[:, :, tt * 128:tt * 128 + nr], psbf[:, :].rearrange("p (h t) -> p h t", h=8)[:, :, 0:nr]), [PS[7]], [QT])
            for h in range(8):
                A("pe", lambda e, h=h, nr=nr: e.transpose(psbf[:, h * 128:h * 128 + nr], qb16.t[0:nr, 1024 + h * 128:1024 + (h + 1) * 128],
                                                          identb.t[0:nr, 0:nr]), [qb16, identb], [PS[7]])
            if tt == 0:
                handoff([XT[1]], [QSTH])
            A("dve", lambda e, tt=tt, nr=nr: e.tensor_copy(
                QST.t[:, :, tt * 128:tt * 128 + nr], psbf[:, 0:512].rearrange("p (a t) -> p a t", a=4)[:, :, 0:nr]), [PS[7]], [QST])
            A("dve", lambda e, tt=tt, nr=nr: e.tensor_copy(
                QSTH.t[:, :, tt * 128:tt * 128 + nr], psbf[:, 512:1024].rearrange("p (a t) -> p a t", a=4)[:, :, 0:nr]), [PS[7]], [QSTH])

        handoff([qb16, A2], [MQ, MT])
        if use_amask:
            S.dma("sp", lambda e: e.dma_start(out=A2.t[:, 3200:3840], in_=amask[halo_col]), writes=[MQ])
            handoff([kb16], [amk])
            A("dve", lambda e: e.tensor_copy(amk.t[:], A2.t[:, 3200:3840]), [MQ], [amk])

        def score_tile(t):
            r0, nq = tiles[t]
            ks = key_sizes_fn(t)
            L = sum(ks)
            q0 = t * 128
            for h in range(8):
                A("dve", lambda e, h=h, nq=nq, t=t: e.tensor_scalar(
                    out=DG.t[0:nq, h, 0:nq], in0=ident.t[0:nq, 0:nq], scalar1=wsa.t[0:nq, t, h:h + 1], scalar2=None, op0=ALU.mult),
                  [ident, wsa], [DG])
            items = []
            k0 = 0
            while k0 < L:
                kn = min(512, L - k0)
                for h in range(8):
                    items.append((k0, kn, h))
                k0 += kn
            n_it = len(items)

            DB = (0, 1, 3, 4)

            def dots(i):
                k0, kn, h = items[i]
                bank = DB[i % 4]
                qs_ = QST if h < 4 else QSTH
                A("pe", lambda e: e.matmul(
                    pst[0:nq, bank, 0:kn], qs_.t[:, h % 4, q0:q0 + nq], KIT.t[:, k0:k0 + kn], start=True, stop=True),
                  [qs_, KIT], [PS[bank]])
                rr = RR[i % 4]
                if i % 2 == 0:
                    A("act", lambda e: e.activation(out=rr.t[0:nq, 0:kn], in_=pst[0:nq, bank, 0:kn], func=AF.Relu), [PS[bank]], [rr])
                else:
                    A("dve", lambda e: e.tensor_scalar(out=rr.t[0:nq, 0:kn], in0=pst[0:nq, bank, 0:kn], scalar1=0.0, scalar2=None, op0=ALU.max),
                      [PS[bank]], [rr])

            def accm(i):
                k0, kn, h = items[i]
                rr = RR[i % 4]
                scb = (2, 5)[(i // 8) % 2]
                A("pe", lambda e: e.matmul(pst[0:nq, scb, 0:kn], DG.t[0:nq, h, 0:nq], rr.t[0:nq, 0:kn], start=(h == 0), stop=(h == 7)),
                  [DG, rr], [PS[scb]])
                if h == 7:
                    A("act", lambda e: e.activation(out=SCR.t[0:nq, k0:k0 + kn], in_=pst[0:nq, scb, 0:kn], func=AF.Copy), [PS[scb]], [SCR])

            for i in range(min(3, n_it)):
                dots(i)
            for i in range(n_it):
                if i + 3 < n_it:
                    dots(i + 3)
                accm(i)
            return L

        def thresh_tile(t, L):
            r0, nq = tiles[t]
            ks = key_sizes_fn(t)
            sc = SCR.t[0:nq, 0:L]
            c = lambda i: sm.t[0:nq, i:i + 1]
            A("dve", lambda e: e.tensor_reduce(out=c(8), in_=sc, op=ALU.max, axis=AX.X), [SCR], [sm])
            A("dve", lambda e: e.tensor_reduce(out=c(9), in_=sc, op=ALU.min, axis=AX.X), [SCR], [sm])
            A("dve", lambda e: e.tensor_tensor(out=c(10), in0=c(8), in1=c(9), op=ALU.subtract), [sm], [sm])
            A("dve", lambda e: e.tensor_scalar(out=c(10), in0=c(10), scalar1=0.505, scalar2=1e-6, op0=ALU.mult, op1=ALU.add), [sm], [sm])
            A("dve", lambda e: e.tensor_tensor(out=c(11), in0=c(8), in1=c(9), op=ALU.add), [sm], [sm])
            A("dve", lambda e: e.tensor_scalar(out=c(11), in0=c(11), scalar1=0.5, scalar2=None, op0=ALU.mult), [sm], [sm])
            A("dve", lambda e: e.tensor_scalar(out=stp.t[0:nq, 0, :], in0=pw.t[0:nq, :], scalar1=c(10), scalar2=None, op0=ALU.mult), [pw, sm], [stp])
            A("dve", lambda e: e.tensor_scalar(out=stp.t[0:nq, 1, :], in0=stp.t[0:nq, 0, :], scalar1=2.0, scalar2=None, op0=ALU.mult), [stp], [stp])
            A("dve", lambda e: e.tensor_scalar(out=stp.t[0:nq, 2, :], in0=stp.t[0:nq, 0, :], scalar1=-1.0, scalar2=None, op0=ALU.mult), [stp], [stp])
            if use_amask:
                A("dve", lambda e: e.tensor_tensor(out=SCR.t[0:nq, L - 640:L], in0=SCR.t[0:nq, L - 640:L], in1=amk.t[0:nq, :], op=ALU.add),
                  [SCR, amk], [SCR])
            for k in range(NIT):
                A("dve", lambda e: e.tensor_scalar(out=junk.t[0:nq, 0:L], in0=sc, scalar1=c(11), scalar2=None,
                                                   op0=ALU.is_ge, op1=ALU.add, accum_out=c(12)), [SCR, sm], [junk, sm])
                A("dve", lambda e, k=k: e.tensor_scalar(out=c(14), in0=c(12), scalar1=TOPK - 0.5, scalar2=stp.t[0:nq, 1, k:k + 1],
                                                        op0=ALU.is_ge, op1=ALU.mult), [sm, stp], [sm])
                A("dve", lambda e, k=k: e.tensor_scalar(out=c(11), in0=c(14), scalar1=c(11), scalar2=stp.t[0:nq, 2, k:k + 1],
                                                        op0=ALU.add, op1=ALU.add), [sm, stp], [sm])
            A("dve", lambda e: e.tensor_tensor(out=c(11), in0=c(11), in1=stp.t[0:nq, 2, NIT - 1:NIT], op=ALU.add), [sm, stp], [sm])
            A("dve", lambda e: e.tensor_scalar(out=MQ.t[0:nq, 0:L], in0=sc, scalar1=c(11), scalar2=MASKV, op0=ALU.is_lt, op1=ALU.mult),
              [SCR, sm], [MQ])
            nk = len(ks)
            kt = 0
            while kt < nk:
                nb_ = min(8, nk - kt)
                for j in range(nb_):
                    kk = kt + j
                    A("pe", lambda e, j=j, kk=kk: e.transpose(psbf[0:ks[kk], j * 128:j * 128 + nq], MQ.t[0:nq, kk * 128:kk * 128 + ks[kk]],
                                                              identb.t[0:nq, 0:nq]), [MQ, identb], [PS[7]])
                A("dve", lambda e, kt=kt, nb_=nb_: e.tensor_copy(
                    MT.t[:, kt:kt + nb_, 0:nq], psbf[:, 0:nb_ * 128].rearrange("p (k q) -> p k q", q=128)[:, :, 0:nq]), [PS[7]], [MT])
                kt += nb_

        def attend_tile(t):
            r0, nq = tiles[t]
            ks = key_sizes_fn(t)
            q0 = t * 128
            items = [(g, kk, kn) for g in range(2) for kk, kn in enumerate(ks)]
            n_it = len(items)

            def st_(i):
                g, kk, kn = items[i]
                sb_ = 3 + (i % 2)
                pt = PT[i % 2]
                A("pe", lambda e: e.matmul(
                    pst[0:kn, sb_, 0:4 * nq].rearrange("p (h q) -> p h q", h=4), KT.t[:, g, kk * 128:kk * 128 + kn],
                    QT.t[:, 4 * g:4 * g + 4, q0:q0 + nq], start=True, stop=False), [KT, QT], [PS[sb_]])
                A("pe", lambda e: e.matmul(
                    pst[0:kn, sb_, 0:4 * nq].rearrange("p (h q) -> p h q", h=4), identb.t[0:kn, 0:kn],
                    MT.t[0:kn, kk, 0:nq].unsqueeze(1).to_broadcast([kn, 4, nq]), start=False, stop=True), [identb, MT], [PS[sb_]])
                A("act", lambda e: e.activation(out=pt.t[0:kn, 0:4 * nq], in_=pst[0:kn, sb_, 0:4 * nq], func=AF.Exp, scale=ASCALE),
                  [PS[sb_]], [pt])

            def pv_(i):
                g, kk, kn = items[i]
                pt = PT[i % 2]
                ob, db = (5, 6) if (g == 0 or ATT_OLD) else (0, 1)
                A("pe", lambda e: e.matmul(pst[:, ob, 0:4 * nq], VV.t[0:kn, kk, g * 128:(g + 1) * 128], pt.t[0:kn, 0:4 * nq],
                                           start=(kk == 0), stop=(kk == len(ks) - 1)), [VV, pt], [PS[ob]])
                A("pe", lambda e: e.matmul(pst[:, db, 0:4 * nq], onesb.t[0:kn, :], pt.t[0:kn, 0:4 * nq],
                                           start=(kk == 0), stop=(kk == len(ks) - 1)), [onesb, pt], [PS[db]])

            st_(0)
            for i in range(n_it):
                if i + 1 < n_it:
                    st_(i + 1)
                pv_(i)
                if ATT_OLD and items[i][1] == len(ks) - 1:
                    attend_fin(t, [items[i][0]])

        def attend_fin(t, gs=(0, 1)):
            r0, nq = tiles[t]
            q0 = t * 128
            for g in gs:
                ob, db = (5, 6) if (g == 0 or ATT_OLD) else (0, 1)
                A("dve", lambda e, db=db: e.reciprocal(out=rec.t[:, 0:4 * nq], in_=pst[:, db, 0:4 * nq]), [PS[db]], [rec])
                A("dve", lambda e, g=g, ob=ob: e.tensor_tensor(
                    out=OX.t[:, 4 * g:4 * g + 4, q0:q0 + nq], in0=pst[:, ob, 0:4 * nq].rearrange("p (h q) -> p h q", h=4),
                    in1=rec.t[:, 0:4 * nq].rearrange("p (h q) -> p h q", h=4), op=ALU.mult), [PS[ob], rec], [OX])

        Ls = {0: score_tile(0)}
        thresh_tile(0, Ls[0])
        for t in range(nt):
            if t + 1 < nt:
                Ls[t + 1] = score_tile(t + 1)
            attend_tile(t)
            if t + 1 < nt:
                thresh_tile(t + 1, Ls[t + 1])
            if not ATT_OLD:
                attend_fin(t)

        handoff([QSTH], [XT[1]])
        make_hT()
        bankc = [0]

        def proj_fm(wsrc, c0, rhs_fn, rhsB, post):
            for hh in range(2):
                slot = wload(wsrc[:, c0 + hh * 512:c0 + (hh + 1) * 512])
                for jj in range(4):
                    j = hh * 4 + jj
                    bank = bankc[0] % 4
                    bankc[0] += 1
                    for kc in range(8):
                        A("pe", lambda e, kc=kc, slot=slot, bank=bank, jj=jj: e.matmul(
                            pst[:, bank, 0:ntok], slot.t[:, kc, jj * 128:(jj + 1) * 128], rhs_fn(kc), start=(kc == 0), stop=(kc == 7)),
                          [slot] + (rhsB if isinstance(rhsB, list) else [rhsB]), [PS[bank]])
                    post(j, bank)

        def post_sig(j, bank):
            A("act", lambda e, j=j, bank=bank: e.activation(out=SG.t[:, j, 0:ntok], in_=pst[:, bank, 0:ntok], func=AF.Sigmoid), [PS[bank]], [SG])

        def post_m1(j, bank):
            A("dve", lambda e, j=j, bank=bank: e.tensor_tensor(out=A3.t[:, j, 0:ntok], in0=pst[:, bank, 0:ntok], in1=SG.t[:, j, 0:ntok], op=ALU.mult),
              [PS[bank], SG], [A3])

        def post_m2(j, bank):
            A("dve", lambda e, j=j, bank=bank: e.tensor_tensor(out=acc.t[:, 0:ntok], in0=pst[:, bank, 0:ntok], in1=SG.t[:, j, 0:ntok], op=ALU.mult),
              [PS[bank], SG], [acc])
            A("dve", lambda e, j=j: e.tensor_tensor(out=A3.t[:, j, 0:ntok], in0=A3f[:, j * 512:j * 512 + ntok], in1=acc.t[:, 0:ntok], op=ALU.add),
              [A3, acc], [A3])

        handoff([MT, MQ], [SG])
        handoff(RR + [DG], [A3])
        proj_fm(w_in, C_GATT, lambda kc: HT.t[:, kc, T0:T0 + ntok], HT_TOK, post_sig)
        proj_fm(w_att, 0, lambda kc: OX.t[:, kc, 0:ntok], OX, post_m1)

        handoff([QT, QST], [ub, sg, acc])
        handoff([SG], [lnt, uo])
        for hh in range(2):
            slot_a = wload(w_in[:, C_GA + hh * 512:C_GA + (hh + 1) * 512])
            slot_b = wload(w_in[:, C_GB + hh * 512:C_GB + (hh + 1) * 512])
            for jj in range(4):
                j = hh * 4 + jj
                for (slot, bank, hc) in ((slot_a, 0, 0), (slot_b, 1, 32)):
                    for kc in range(8):
                        A("pe", lambda e, kc=kc, slot=slot, bank=bank, jj=jj: e.matmul(
                            pst[:, bank, 0:ntok], slot.t[:, kc, jj * 128:(jj + 1) * 128], HT.t[:, kc, T0:T0 + ntok],
                            start=(kc == 0), stop=(kc == 7)), [slot] + HT_TOK, [PS[bank]])
                    if halo_src is not None:
                        for kc in range(8):
                            A("pe", lambda e, kc=kc, slot=slot, hc=hc, jj=jj: e.matmul(
                                pst[:, 2, hc:hc + 32], slot.t[:, kc, jj * 128:(jj + 1) * 128], HT.t[:, kc, 0:32],
                                start=(kc == 0), stop=(kc == 7)), [slot] + HTres(0), [PS[2]])
                A("act", lambda e: e.activation(out=sg.t[:, T0:T0 + ntok], in_=pst[:, 1, 0:ntok], func=AF.Sigmoid), [PS[1]], [sg])
                A("dve", lambda e: e.tensor_tensor(out=ubr.t[:, T0:T0 + ntok], in0=pst[:, 0, 0:ntok], in1=sg.t[:, T0:T0 + ntok], op=ALU.mult),
                  [PS[0], sg], [ubr])
                if halo_src is not None:
                    A("act", lambda e: e.activation(out=sg.t[:, 0:32], in_=pst[:, 2, 32:64], func=AF.Sigmoid), [PS[2]], [sg])
                    A("dve", lambda e: e.scalar_tensor_tensor(out=ubr.t[:, 0:32], in0=pst[:, 2, 0:32], scalar=hvt.t[:, halo_col:halo_col + 1],
                                                              in1=sg.t[:, 0:32], op0=ALU.mult, op1=ALU.mult), [PS[2], sg, hvt], [ubr])
                else:
                    S.dma("sp", lambda e, j=j: e.dma_start(out=ubr.t[:, 0:32], in_=scvT[j * 128:(j + 1) * 128, :]), writes=[ubr])
                if conv_out is not None:
                    A("pe", lambda e, jj=jj: e.transpose(pst[0:32, 4, jj * 128:jj * 128 + 128],
                                                         ubf[:, T0 + conv_tok0:T0 + conv_tok0 + 32], ident.t[:, :]), [ubr, ident], [PS[4]])
                    A("act", lambda e, j=j, jj=jj: e.activation(out=uo.t[0:32, j * 128:(j + 1) * 128],
                                                                in_=pst[0:32, 4, jj * 128:jj * 128 + 128], func=AF.Copy), [PS[4]], [uo])
                NPE = 22
                for k in range(NPE):
                    if k % 2 == 0:
                        dg_ = dgc[(k // 2) % 2]
                        A("dve", lambda e, j=j, k=k, dg_=dg_: e.tensor_scalar(out=dg_.t[:, :], in0=ident.t[:, :], scalar1=wdw.t[:, j, k:k + 1],
                                                                              scalar2=None, op0=ALU.mult), [ident, wdw], [dg_])
                    else:
                        dg_ = dgc[2 + (k // 2) % 2]
                        A("act", lambda e, j=j, k=k, dg_=dg_: e.activation(out=dg_.t[:, :], in_=ident.t[:, :], func=AF.Identity,
                                                                           scale=wdw.t[:, j, k:k + 1]), [ident, wdw], [dg_])
                    A("pe", lambda e, k=k, dg_=dg_: e.matmul(pst[:, 3, 0:ntok], dg_.t[:, :], ubr.t[:, 2 + k:2 + k + ntok],
                                                             start=(k == 0), stop=(k == NPE - 1)), [dg_, ubr], [PS[3]])
                    w = NPE + k
                    if w < 31:
                        if k == 0:
                            A("dve", lambda e, j=j, w=w: e.tensor_scalar(out=acc.t[:, 0:ntok], in0=ubf[:, 2 + w:2 + w + ntok], scalar1=wdw.t[:, j, w:w + 1],
                                                                         scalar2=cvc.t[:, 0, j:j + 1], op0=ALU.mult, op1=ALU.add), [ubr, wdw, cvc], [acc])
                        else:
                            A("dve", lambda e, j=j, w=w: e.scalar_tensor_tensor(
                                out=acc.t[:, 0:ntok], in0=ubf[:, 2 + w:2 + w + ntok], scalar=wdw.t[:, j, w:w + 1], in1=acc.t[:, 0:ntok],
                                op0=ALU.mult, op1=ALU.add), [ubr, wdw, acc], [acc])
                A("dve", lambda e, j=j: e.tensor_tensor(out=OX.t[:, j, 0:ntok], in0=pst[:, 3, 0:ntok], in1=acc.t[:, 0:ntok], op=ALU.add),
                  [PS[3], acc], [OX])
                A("act", lambda e, j=j: e.activation(out=ysq.t[:, 0:ntok], in_=OXf[:, j * 512:j * 512 + ntok], func=AF.Square), [OX], [ysq])
                A("pe", lambda e, j=j: e.matmul(pst[:, 5, 0:ntok], onesr.t[:, :], OX.t[:, j, 0:ntok], start=(j == 0), stop=(j == 7)),
                  [onesr, OX], [PS[5]])
                A("pe", lambda e, j=j: e.matmul(pst[:, 6, 0:ntok], onesr.t[:, :], ysq.t[:, 0:ntok], start=(j == 0), stop=(j == 7)),
                  [onesr, ysq], [PS[6]])
        if conv_out is not None:
            out_toks.append(S.dma("pool", lambda e: e.dma_start(out=conv_out, in_=uo.t[0:32, :]), reads=[uo]))
        mean_ = lnt.t[:, 0, 0:ntok]; rstd_ = lnt.t[:, 1, 0:ntok]; tmp_ = lnt.t[:, 2, 0:ntok]
        A("dve", lambda e: e.tensor_scalar(out=mean_, in0=pst[:, 5, 0:ntok], scalar1=1.0 / D, scalar2=None, op0=ALU.mult), [PS[5]], [lnt])
        A("dve", lambda e: e.tensor_tensor(out=tmp_, in0=mean_, in1=mean_, op=ALU.mult), [lnt], [lnt])
        A("dve", lambda e: e.scalar_tensor_tensor(out=rstd_, in0=pst[:, 6, 0:ntok], scalar=1.0 / D, in1=tmp_, op0=ALU.mult, op1=ALU.subtract),
          [PS[6], lnt], [lnt])
        A("act", lambda e: e.activation(out=rstd_, in_=rstd_, func=AF.Sqrt, bias=LN_EPS, scale=1.0), [lnt], [lnt])
        A("dve", lambda e: e.reciprocal(out=rstd_, in_=rstd_), [lnt], [lnt])
        for j in range(8):
            A("dve", lambda e, j=j: e.tensor_tensor(out=tmp_, in0=OXf[:, j * 512:j * 512 + ntok], in1=mean_, op=ALU.subtract), [OX, lnt], [lnt])
            A("dve", lambda e: e.tensor_tensor(out=tmp_, in0=tmp_, in1=rstd_, op=ALU.mult), [lnt], [lnt])
            A("act", lambda e, j=j: e.activation(out=OX.t[:, j, 0:ntok], in_=tmp_, func=AF.Silu, scale=cvc.t[:, 1, j:j + 1],
                                                 bias=cvc.t[:, 2, j:j + 1]), [lnt, cvc], [OX])

        handoff([lnt, uo], [SG])
        proj_fm(w_in, C_GCONV, lambda kc: HT.t[:, kc, T0:T0 + ntok], HT_TOK, post_sig)
        proj_fm(w_co, 0, lambda kc: OX.t[:, kc, 0:ntok], OX, post_m2)

        handoff([SCR], BC)
        S.dma("sp", lambda e: e.dma_start(out=BC[0].t, in_=gview[0 * 2 + seq:0 * 2 + seq + 1, :].to_broadcast([128, D])), reads=[gs_res], writes=[BC[0]])
        S.dma("sp", lambda e: e.dma_start(out=BC[1].t, in_=lnrow[0:1, :].to_broadcast([128, D])), writes=[BC[1]])
        S.dma("sp", lambda e: e.dma_start(out=BC[2].t, in_=lnrow[1:2, :].to_broadcast([128, D])), writes=[BC[2]])
        wo_slots = [wload(w_o[:, dh * 512:(dh + 1) * 512]) for dh in range(2)]

        def wo_tile(tt):
            r0, nr = tiles[tt]
            for dh in range(2):
                slot = wo_slots[dh]
                bank = dh
                for kc in range(8):
                    A("pe", lambda e, kc=kc, slot=slot, bank=bank: e.matmul(
                        pst[0:nr, bank, :], A3.t[:, kc, tt * 128:tt * 128 + nr], slot.t[:, kc, :], start=(kc == 0), stop=(kc == 7)),
                      [A3, slot], [PS[bank]])
                A("dve", lambda e, bank=bank, dh=dh: e.tensor_tensor(
                    out=X1r[0:nr, tt, dh * 512:(dh + 1) * 512], in0=pst[0:nr, bank, :], in1=BC[0].t[0:nr, dh * 512:(dh + 1) * 512], op=ALU.mult),
                  [PS[bank], BC[0]], [OX])

        def ln_tile(tt):
            r0, nr = tiles[tt]
            xb_ = load_x(xsrc[r0:r0 + nr, :], nr)
            x1w = X1r[0:nr, tt, :]
            x1rd = X1f[0:nr, tt, :]
            A("dve", lambda e: e.scalar_tensor_tensor(
                out=x1w, in0=xb_.t[0:nr, :], scalar=ALPHA, in1=x1rd, op0=ALU.mult, op1=ALU.add), [xb_, OX], [OX])
            bn_ln(x1rd, nr, OX)
            A("dve", lambda e: e.tensor_scalar(out=x1w, in0=x1rd, scalar1=mv.t[0:nr, 0:1], scalar2=sm.t[0:nr, 0:1],
                                               op0=ALU.subtract, op1=ALU.mult), [OX, mv, sm], [OX])
            A("dve", lambda e: e.tensor_tensor(out=x1w, in0=x1rd, in1=BC[1].t[0:nr, :], op=ALU.mult), [OX, BC[1]], [OX])
            A("dve", lambda e: e.tensor_tensor(out=x1w, in0=x1rd, in1=BC[2].t[0:nr, :], op=ALU.add), [OX, BC[2]], [OX])
            transpose_mod(OX, nr, HT, T0 + tt * 128, 1, seq, src_t=X1f[:, tt, :], g=1 + tt)

        wo_tile(0)
        for tt in range(nt):
            if tt + 1 < nt:
                wo_tile(tt + 1)
            ln_tile(tt)

        handoff(BC, [sgu])
        handoff([SG], [YA])
        for tt, (r0, nr) in enumerate(tiles):
            for kc in range(8):
                A("pe", lambda e, kc=kc, tt=tt, nr=nr: e.matmul(
                    pst[0:nr, 6, 0:20], HT.t[:, kc, T0 + tt * 128:T0 + tt * 128 + nr].bitcast(F32), wrt.t[:, kc, :],
                    start=(kc == 0), stop=(kc == 7)), HTres(1 + tt) + [wrt], [PS[6]])
            R = lambda a_, b_, nr=nr: rt.t[0:nr, a_:b_]
            A("dve", lambda e, nr=nr, R=R: e.tensor_tensor(out=R(0, 20), in0=pst[0:nr, 6, 0:20], in1=brt.t[0:nr, :], op=ALU.add), [PS[6], brt], [rt])
            A("dve", lambda e, R=R: e.tensor_reduce(out=R(20, 21), in_=R(0, 4), op=ALU.max, axis=AX.X), [rt], [rt])
            A("dve", lambda e, R=R: e.tensor_scalar(out=R(24, 28), in0=R(0, 4), scalar1=R(20, 21), scalar2=None, op0=ALU.subtract), [rt], [rt])
            A("act", lambda e, R=R: e.activation(out=R(28, 32), in_=R(24, 28), func=AF.Exp, accum_out=R(21, 22)), [rt], [rt])
            A("dve", lambda e, R=R: e.reciprocal(out=R(21, 22), in_=R(21, 22)), [rt], [rt])
            A("dve", lambda e, R=R: e.tensor_scalar(out=R(24, 28), in0=R(0, 4), scalar1=R(20, 21), scalar2=BIG, op0=ALU.is_ge, op1=ALU.mult), [rt], [rt])
            A("dve", lambda e, R=R: e.tensor_scalar(out=R(24, 28), in0=R(24, 28), scalar1=-BIG, scalar2=None, op0=ALU.add), [rt], [rt])
            A("dve", lambda e, R=R, nr=nr: e.tensor_tensor(
                out=R(32, 48).rearrange("p (g k) -> p g k", g=4), in0=R(4, 20).rearrange("p (g k) -> p g k", g=4),
                in1=R(24, 28).unsqueeze(2).to_broadcast([nr, 4, 4]), op=ALU.add), [rt], [rt])
            A("dve", lambda e, R=R: e.tensor_reduce(out=R(22, 23), in_=R(32, 48), op=ALU.max, axis=AX.X), [rt], [rt])
            A("dve", lambda e, R=R: e.tensor_scalar(out=R(48, 64), in0=R(32, 48), scalar1=R(22, 23), scalar2=None, op0=ALU.is_ge), [rt], [rt])
            A("dve", lambda e, R=R: e.scalar_tensor_tensor(out=R(64, 80), in0=R(48, 64), scalar=-BIG, in1=R(32, 48), op0=ALU.mult, op1=ALU.add), [rt], [rt])
            A("dve", lambda e, R=R: e.tensor_reduce(out=R(23, 24), in_=R(64, 80), op=ALU.max, axis=AX.X), [rt], [rt])
            A("dve", lambda e, R=R: e.tensor_scalar(out=R(80, 96), in0=R(64, 80), scalar1=R(23, 24), scalar2=None, op0=ALU.is_ge), [rt], [rt])
            A("dve", lambda e, R=R: e.tensor_tensor(out=R(24, 25), in0=R(23, 24), in1=R(22, 23), op=ALU.subtract), [rt], [rt])
            A("act", lambda e, R=R: e.activation(out=R(25, 26), in_=R(24, 25), func=AF.Exp), [rt], [rt])
            A("dve", lambda e, R=R: e.tensor_scalar(out=R(26, 27), in0=R(25, 26), scalar1=1.0, scalar2=None, op0=ALU.add), [rt], [rt])
            A("dve", lambda e, R=R: e.reciprocal(out=R(26, 27), in_=R(26, 27)), [rt], [rt])
            A("dve", lambda e, R=R: e.tensor_tensor(out=R(27, 28), in0=R(25, 26), in1=R(26, 27), op=ALU.mult), [rt], [rt])
            A("dve", lambda e, R=R: e.tensor_scalar(out=R(26, 28), in0=R(26, 28), scalar1=R(21, 22), scalar2=None, op0=ALU.mult), [rt], [rt])
            A("dve", lambda e, R=R: e.tensor_scalar(out=R(32, 48), in0=R(48, 64), scalar1=R(26, 27), scalar2=None, op0=ALU.mult), [rt], [rt])
            A("dve", lambda e, R=R, nr=nr, tt=tt: e.scalar_tensor_tensor(out=comb.t[0:nr, tt, :], in0=R(80, 96), scalar=R(27, 28),
                                                                         in1=R(32, 48), op0=ALU.mult, op1=ALU.add), [rt], [comb])
        dcnt = [0]
        ACT2 = [B(A3r[:, i * 1024:(i + 1) * 1024].rearrange("p (a b) -> p a b", a=2), "ACT2_%d" % i) for i in range(2)]
        SGU2 = [B(A1.t[:, 3072 + i * 512:3584 + i * 512], "sgu%d" % i) for i in range(2)]
        for o_ in ACT2:
            o_.r = Res(o_.r.name)
        handoff([A3], ACT2)
        handoff(BC + [sgu], SGU2)
        dn_views = {}

        def gu_(ex):
            slot_gu = WS[ex % 2]
            S.dma("sp", lambda e: e.dma_start(out=slot_gu.t[:, :, :], in_=w_gu[ex].rearrange("(c p) n -> p c n", p=128)), writes=[slot_gu])
            slot_dn = WSH[ex % 2]
            hk = 4 * (ex % 2)
            dnv = WS[2].t[:, hk:hk + 4, :].rearrange("p a b -> p (a b)").rearrange("p (f n) -> p f n", f=2)
            S.dma("sp", lambda e: e.dma_start(out=dnv, in_=w_dn[ex].rearrange("(f p) n -> p f n", p=128)), writes=[slot_dn])
            dn_views[ex] = (slot_dn, dnv)
            at = ACT2[ex % 2]
            for fc in range(2):
                for (c0, bank) in ((fc * 128, 0 + fc), (256 + fc * 128, 2 + fc)):
                    for kc in range(8):
                        A("pe", lambda e, kc=kc, c0=c0, bank=bank: e.matmul(
                            pst[:, bank, 0:ntok], slot_gu.t[:, kc, c0:c0 + 128], HT.t[:, kc, T0:T0 + ntok], start=(kc == 0), stop=(kc == 7)),
                          [slot_gu] + HT_TOK, [PS[bank]])
                su = SGU2[fc]
                A("act", lambda e, fc=fc, su=su: e.activation(out=su.t[:, 0:ntok], in_=pst[:, 0 + fc, 0:ntok], func=AF.Silu), [PS[0 + fc]], [su])
                A("dve", lambda e, fc=fc, su=su: e.tensor_tensor(out=at.t[:, fc, 0:ntok], in0=pst[:, 2 + fc, 0:ntok], in1=su.t[:, 0:ntok], op=ALU.mult),
                  [PS[2 + fc], su], [at])

        def down_(ex):
            slot_dn, dnv = dn_views[ex]
            at = ACT2[ex % 2]
            for tt, (r0, nr) in enumerate(tiles):
                for dh in range(2):
                    bank = 4 + dcnt[0] % 3
                    dcnt[0] += 1
                    for fc in range(2):
                        A("pe", lambda e, fc=fc, tt=tt, nr=nr, dh=dh, bank=bank: e.matmul(
                            pst[0:nr, bank, :], at.t[:, fc, tt * 128:tt * 128 + nr], dnv[:, fc, dh * 512:(dh + 1) * 512],
                            start=(fc == 0), stop=(fc == 1)), [at, slot_dn], [PS[bank]])
                    ya = YA.t[0:nr, tt, dh * 512:(dh + 1) * 512]
                    if ex == 0:
                        A("dve", lambda e, nr=nr, tt=tt, bank=bank, ya=ya: e.tensor_scalar(
                            out=ya, in0=pst[0:nr, bank, :], scalar1=comb.t[0:nr, tt, ex:ex + 1], scalar2=None, op0=ALU.mult), [PS[bank], comb], [YA])
                    else:
                        A("dve", lambda e, nr=nr, tt=tt, bank=bank, ya=ya: e.scalar_tensor_tensor(
                            out=ya, in0=pst[0:nr, bank, :], scalar=comb.t[0:nr, tt, ex:ex + 1], in1=ya, op0=ALU.mult, op1=ALU.add),
                          [PS[bank], comb, YA], [YA])

        WSH = [B(None, "WSH0"), B(None, "WSH1")]
        handoff([WS[2]], WSH)
        gu_(0)
        for ex in range(16):
            if ex + 1 < 16:
                gu_(ex + 1)
            down_(ex)
        handoff(WSH, [WS[2]])
        handoff(ACT2, [A3])
        handoff(SGU2, [sgu])
        if prefetch is not None:
            prefetch()
        handoff([sgu], BC)
        S.dma("sp", lambda e: e.dma_start(out=BC[0].t, in_=gview[1 * 2 + seq:1 * 2 + seq + 1, :].to_broadcast([128, D])), reads=[gs_res], writes=[BC[0]])
        S.dma("sp", lambda e: e.dma_start(out=BC[1].t, in_=lnrow[2:3, :].to_broadcast([128, D])), writes=[BC[1]])
        S.dma("sp", lambda e: e.dma_start(out=BC[2].t, in_=lnrow[3:4, :].to_broadcast([128, D])), writes=[BC[2]])
        for tt, (r0, nr) in enumerate(tiles):
            ya = YA.t[0:nr, tt, :]
            A("dve", lambda e, nr=nr, ya=ya: e.tensor_tensor(out=ya, in0=ya, in1=BC[0].t[0:nr, :], op=ALU.mult), [YA, BC[0]], [YA])
            A("dve", lambda e, nr=nr, tt=tt, ya=ya: e.scalar_tensor_tensor(out=ya, in0=X1f[0:nr, tt, :], scalar=ALPHA, in1=ya, op0=ALU.mult, op1=ALU.add),
              [OX, YA], [YA])
            bn_ln(ya, nr, YA)
            A("dve", lambda e, nr=nr, ya=ya: e.tensor_scalar(out=ya, in0=ya, scalar1=mv.t[0:nr, 0:1], scalar2=sm.t[0:nr, 0:1],
                                                             op0=ALU.subtract, op1=ALU.mult), [YA, mv, sm], [YA])
            A("dve", lambda e, nr=nr, ya=ya: e.tensor_tensor(out=ya, in0=ya, in1=BC[1].t[0:nr, :], op=ALU.mult), [YA, BC[1]], [YA])
            A("dve", lambda e, nr=nr, ya=ya: e.tensor_tensor(out=ya, in0=ya, in1=BC[2].t[0:nr, :], op=ALU.add), [YA, BC[2]], [YA])
            out_toks.append(S.dma("pool", lambda e, r0=r0, nr=nr, ya=ya: e.dma_start(out=y_out[r0:r0 + nr, :], in_=ya), reads=[YA]))
        handoff(BC, [A1])
        handoff([ub, sg, acc], [QT])
        handoff(PT + [rec], kvsb)
        handoff([YA], [qb16])
        handoff([A3], RR + [DG])

    NSTP = int(os.environ.get("NSTP", "4"))
    ATT_OLD = int(os.environ.get("ATT_OLD", "0"))
    def cache_prep():
        handoff(PT + [rec], kvsb)
        handoff([amk], [kb16])
        for n in range(8):
            kvb = kvsb[n % 2]
            S.dma("sp", lambda e, n=n, kvb=kvb: e.dma_start(out=kvb.t[:, 0:256], in_=ck[n * 128:(n + 1) * 128, :]), writes=[kvb])
            S.dma("sp", lambda e, n=n, kvb=kvb: e.dma_start(out=kvb.t[:, 256:512], in_=cv[n * 128:(n + 1) * 128, :]), writes=[kvb])
            S.dma("sp", lambda e, n=n, kvb=kvb: e.dma_start(out=kvb.t[:, 512:576], in_=cki[n * 128:(n + 1) * 128, :]), writes=[kvb])
            kv_store(kvb, 128, n)

    st_tiles = lambda i: [(i * 512 + t * 128, 128) for t in range(4)]
    for i in range(NSTP):
        nxt = cache_prep if (i == 3 and stage > 2) else None
        if i + 1 < NSTP:
            nxt = (lambda i=i: make_hT_for(st_tiles(i + 1), xo, xh[(i + 1) * 32:(i + 2) * 32, :], 0))
        process_st(512, st_tiles(i), xo, xh[i * 32:(i + 1) * 32, :], i, rope_own, 0,
                   (lambda t, i=i: [128] * (8 * i + 5 + t)), True, yo,
                   (cvo if i == 3 else None), 480, hT_done=(i > 0), prefetch=nxt)

    if stage == 2:
        return finish()

    S.dma("sp", lambda e: e.dma_start(out=WS[0].t[:, :, 0:512], in_=w_kv[:, 0:512].rearrange("(c p) n -> p c n", p=128)), writes=[WS[0]])
    S.dma("sp", lambda e: e.dma_start(out=WS[1].t[:, :, 0:64], in_=w_kv[:, 512:576].rearrange("(c p) n -> p c n", p=128)), writes=[WS[1]])
    wcount[0] = 2
    xb = load_x(xs, TS)
    transpose_mod(xb, TS, HT, 32, 0, 1, g=1)
    kv_tile2(HT, 32, TS, rope_s, kvs, 8, g=1)
    process_st(TS, [(0, TS)], xs, None, 0, rope_s, 1, (lambda t: [128] * 8 + [TS]), False, ys, cvs, 32)
    return finish()


def _rope_tab(pos):
    pos = np.asarray(pos, dtype=np.float32)
    out = np.zeros((pos.shape[0], 96), np.float32)
    for (half, c0) in ((16, 0), (8, 64)):
        inv = (np.float32(ROPE_THETA) ** (-np.arange(half, dtype=np.float32) / np.float32(half))).astype(np.float32)
        ang = (pos[:, None] * inv[None, :]).astype(np.float32)
        c = np.cos(ang).astype(np.float32)
        s = np.sin(ang).astype(np.float32)
        out[:, c0:c0 + half] = c
        out[:, c0 + half:c0 + 2 * half] = c
        out[:, c0 + 2 * half:c0 + 3 * half] = -s
        out[:, c0 + 3 * half:c0 + 4 * half] = s
    return out


OWN = ((0, 3, 4, 7), (1, 2, 5, 6))
_NC_CACHE = {}


def _colT(v):
    return np.ascontiguousarray(np.asarray(v, np.float32).reshape(8, 128).T)


def kernel(x_prompt, x_sample, cache_k, cache_v, cache_idx_k, state_conv, c_prompt, c_sample,
           w_ada, b_ada, w_in, w_att_out, w_dw, b_dw, conv_ln_g, conv_ln_b, w_conv_out, w_o,
           ln1_g, ln1_b, w_group, b_group, w_router, b_router, w_gate_up, w_down, ln2_g, ln2_b, _stage=99):
    f = lambda a: np.ascontiguousarray(np.asarray(a, dtype=np.float32))
    x_prompt = f(x_prompt); x_sample = f(x_sample)
    w_in0 = f(w_in)[0]
    common = {
        "w_ada": f(w_ada)[0], "b_adaT": np.ascontiguousarray(f(b_ada)[0].reshape(48, 128).T),
        "w_in": w_in0, "w_kv": np.ascontiguousarray(np.concatenate([w_in0[:, C_K:C_K + 512], w_in0[:, C_KI:C_KI + 64]], axis=1)), "w_wi": np.ascontiguousarray(w_in0[:, C_WI:C_WI + 8]),
        "w_att": f(w_att_out)[0], "w_co": f(w_conv_out)[0], "w_o": f(w_o)[0],
        "wdwT": np.ascontiguousarray(f(w_dw)[0].reshape(31, 8, 128).transpose(2, 1, 0)),
        "cvec": np.ascontiguousarray(np.stack([_colT(f(b_dw)[0]), _colT(f(conv_ln_g)[0]), _colT(f(conv_ln_b)[0])], axis=1)),
        "lnrow": np.ascontiguousarray(np.stack([f(ln1_g)[0], f(ln1_b)[0], f(ln2_g)[0], f(ln2_b)[0]], axis=0)),
        "w_rt": np.ascontiguousarray(np.concatenate([f(w_group)[0], f(w_router)[0]], axis=1)),
        "b_rt": np.ascontiguousarray(np.concatenate([f(b_group)[0], f(b_router)[0]])[None, :]),
        "w_gu": f(w_gate_up)[0], "w_dn": f(w_down)[0],
        "rope_all": _rope_tab(np.arange(SEQ)), "rope_s": _rope_tab(PAST + np.arange(TS)),
        "ident": np.eye(128, dtype=np.float32),
        "pw2": np.ascontiguousarray(np.tile((0.5 ** (np.arange(NIT) + 1)).astype(np.float32)[None, :], (128, 1))),
    }
    in_maps = []
    for c in range(8):
        b, hf = c // 2, c % 2
        own = OWN[hf]
        xb = x_prompt[b]
        xo = np.concatenate([xb[s * 512:(s + 1) * 512] for s in own], axis=0)
        xh = np.zeros((4, 32, D), np.float32)
        hv = np.zeros((128, 4), np.float32)
        for i, s in enumerate(own):
            if s > 0:
                xh[i] = xb[s * 512 - 32:s * 512]
                hv[:, i] = 1.0
        pos_own = np.concatenate([np.arange(s * 512, (s + 1) * 512) for s in own])
        am = np.zeros((4, 128, 5, 128), np.float32)
        diag = np.zeros((128, 128), np.float32)
        diag[0:64, 64:128] = NEG
        for i, s in enumerate(own):
            if s % 2 == 1:
                am[i, :, 4, :] = diag
            else:
                am[i, :, 0, :] = diag
                am[i, :, 1:, :] = NEG
        scv = np.zeros((32, D), np.float32)
        scv[2:] = f(state_conv)[0, c]
        ccT = np.stack([f(c_prompt)[b], f(c_sample)[c]], axis=0)
        ccT = np.ascontiguousarray(ccT.reshape(2, 8, 128).transpose(2, 1, 0))
        m = dict(common)
        m.update({
            "xa": xb, "xo": np.ascontiguousarray(xo), "xh": np.ascontiguousarray(xh.reshape(128, D)), "hv": hv,
            "xs": x_sample[c], "ck": np.ascontiguousarray(f(cache_k)[0, c].reshape(PAST, 256)),
            "cv": np.ascontiguousarray(f(cache_v)[0, c].reshape(PAST, 256)), "cki": f(cache_idx_k)[0, c],
            "scvT": np.ascontiguousarray(scv.T), "ccT": ccT,
            "rope_own": _rope_tab(pos_own), "amask": np.ascontiguousarray(am.reshape(4, 128, 640)),
        })
        in_maps.append(m)
    if _stage not in _NC_CACHE:
        _NC_CACHE[_stage] = build_program(_stage)
    nc = _NC_CACHE[_stage]
    res = run_bass_kernel_spmd(nc, in_maps, core_ids=list(range(8)))
    R = res.results
    yp = np.zeros((4, SEQ, D), np.float32)
    kp = np.zeros((1, 4, SEQ, 2, 128), np.float32)
    vp = np.zeros((1, 4, SEQ, 2, 128), np.float32)
    kip = np.zeros((1, 4, SEQ, 64), np.float32)
    cp = np.zeros((1, 4, 30, D), np.float32)
    ysa = np.zeros((8, TS, D), np.float32)
    ksa = np.zeros((1, 8, TS, 2, 128), np.float32)
    vsa = np.zeros((1, 8, TS, 2, 128), np.float32)
    kisa = np.zeros((1, 8, TS, 64), np.float32)
    csa = np.zeros((1, 8, 30, D), np.float32)
    for c in range(8):
        b, hf = c // 2, c % 2
        r = R[c]
        for i, s in enumerate(OWN[hf]):
            yp[b, s * 512:(s + 1) * 512] = r["yo"][i * 512:(i + 1) * 512]
            kv = r["kvo"][s * 512:(s + 1) * 512]
            kp[0, b, s * 512:(s + 1) * 512] = kv[:, 0:256].reshape(512, 2, 128)
            vp[0, b, s * 512:(s + 1) * 512] = kv[:, 256:512].reshape(512, 2, 128)
            kip[0, b, s * 512:(s + 1) * 512] = kv[:, 512:576]
        if hf == 0:
            cp[0, b] = r["cvo"][2:32]
        ysa[c] = r["ys"]
        ksa[0, c] = r["kvs"][:, 0:256].reshape(TS, 2, 128)
        vsa[0, c] = r["kvs"][:, 256:512].reshape(TS, 2, 128)
        kisa[0, c] = r["kvs"][:, 512:576]
        csa[0, c] = r["cvs"][2:32]
    return (yp, ysa, kp, vp, kip, cp, ksa, vsa, kisa, csa)
```

```python
import math
from contextlib import ExitStack

import numpy as np
import concourse.bass as bass
import concourse.mybir as mybir
from concourse.bass_utils import run_bass_kernel_spmd

F32 = mybir.dt.float32
F32R = mybir.dt.float32r
BF16 = mybir.dt.bfloat16
AF = mybir.ActivationFunctionType
ALU = mybir.AluOpType
AX = mybir.AxisListType

D = 1024
SEQ = 4096
PAST = 1024
TS = 64
NKC = 8
N_IN = 6216
ALPHA = 2.0 ** 0.25
LN_EPS = 1e-5
NEG = -1.0e30
MASKV = -30000.0
NIT = 13
TOPK = 256
ROPE_THETA = 500000.0
C_Q, C_K, C_V, C_QI, C_KI, C_WI, C_GA, C_GB, C_GATT, C_GCONV = 0, 1024, 1280, 1536, 2048, 2112, 2120, 3144, 4168, 5192
ISCALE = (64 ** -0.5) * (8 ** -0.5)
ASCALE = 128 ** -0.5

ENGS = ("pe", "act", "dve", "pool", "sp")


class Res:
    __slots__ = ("name", "w", "r")

    def __init__(self, name):
        self.name = name
        self.w = None
        self.r = {}


class Tok:
    __slots__ = ("eng", "idx", "sem", "val")

    def __init__(self, eng, idx, sem=None, val=None):
        self.eng = eng
        self.idx = idx
        self.sem = sem
        self.val = val


class Op:
    __slots__ = ("fn", "waits", "inc", "dma_sem")

    def __init__(self, fn):
        self.fn = fn
        self.waits = []
        self.inc = False
        self.dma_sem = None


class Sched:
    def __init__(self, n_dma_sems=8):
        self.ops = {e: [] for e in ENGS}
        self.inc_ord = {e: [] for e in ENGS}
        self.cnt = {e: 0 for e in ENGS}
        self.waited = {e: {} for e in ENGS}
        self.n_dma = n_dma_sems
        self.dma_k = {e: 0 for e in ENGS}
        self.dma_last = {}

    def _resolve(self, tok):
        if tok.sem is not None:
            return tok.sem, tok.val
        e = tok.eng
        if tok.val is not None:
            return ("eng", e), tok.val
        ords = self.inc_ord[e]
        i = tok.idx
        n = len(ords)
        j = i
        while j < n and ords[j] is None:
            j += 1
        if j < n:
            tok.val = ords[j]
        else:
            last = n - 1
            while last > i and (self.ops[e][last].dma_sem is not None or self.ops[e][last].fn is None):
                last -= 1
            self.cnt[e] += 1
            ords[last] = self.cnt[e]
            self.ops[e][last].inc = True
            tok.val = self.cnt[e]
        return ("eng", e), tok.val

    def _add_wait(self, eng, op, tok):
        if tok is None:
            return
        if tok.eng == "pe" and eng == "pe":
            return
        key, val = self._resolve(tok)
        w = self.waited[eng]
        if w.get(key, 0) >= val:
            return
        w[key] = val
        op.waits.append((key, val))

    def _deps(self, eng, op, reads, writes):
        for r in reads:
            self._add_wait(eng, op, r.w)
        for wr in writes:
            self._add_wait(eng, op, wr.w)
            for t in wr.r.values():
                self._add_wait(eng, op, t)

    def add(self, eng, fn, reads=(), writes=()):
        reads = [b.r if hasattr(b, "r") and isinstance(b.r, Res) else b for b in reads]
        writes = [b.r if hasattr(b, "r") and isinstance(b.r, Res) else b for b in writes]
        op = Op(fn)
        self._deps(eng, op, reads, writes)
        idx = len(self.ops[eng])
        self.ops[eng].append(op)
        self.inc_ord[eng].append(None)
        tok = Tok(eng, idx)
        for r in reads:
            r.r[eng] = tok
        for wr in writes:
            wr.w = tok
            wr.r = {}
        return tok

    def dma(self, eng, fn, reads=(), writes=()):
        reads = [b.r if hasattr(b, "r") and isinstance(b.r, Res) else b for b in reads]
        writes = [b.r if hasattr(b, "r") and isinstance(b.r, Res) else b for b in writes]
        op = Op(fn)
        k = self.dma_k[eng]
        self.dma_k[eng] = k + 1
        slot = k % self.n_dma
        key = ("dma", eng, slot)
        prev = self.dma_last.get(key, 0)
        if prev > 0:
            w = self.waited[eng]
            if w.get(key, 0) < 16 * prev:
                w[key] = 16 * prev
                op.waits.append((key, 16 * prev))
        self.dma_last[key] = prev + 1
        self._deps(eng, op, reads, writes)
        op.dma_sem = key
        idx = len(self.ops[eng])
        self.ops[eng].append(op)
        self.inc_ord[eng].append(None)
        tok = Tok(None, idx, sem=key, val=16 * (prev + 1))
        for r in reads:
            r.r[("dma", eng, slot, prev)] = tok
        for wr in writes:
            wr.w = tok
            wr.r = {}
        return tok

    def final_wait(self, eng, toks):
        op = Op(None)
        for t in toks:
            self._add_wait(eng, op, t)
        self.ops[eng].append(op)
        self.inc_ord[eng].append(None)

    def emit(self, nc, stack):
        sems = {}

        def sem(key):
            if key not in sems:
                sems[key] = stack.enter_context(nc.semaphore("s_" + "_".join(str(x) for x in key)))
            return sems[key]

        for e in ENGS:
            if self.cnt[e] > 0:
                sem(("eng", e))
        for key in sorted(self.dma_last.keys(), key=str):
            sem(key)
        block = stack.enter_context(nc.Block())

        def run(engname):
            def body(eng):
                for op in self.ops[engname]:
                    for key, val in op.waits:
                        eng.wait_ge(sem(key), val)
                    if op.fn is None:
                        continue
                    inst = op.fn(eng)
                    if op.dma_sem is not None:
                        inst.then_inc(sem(op.dma_sem), 16)
                    elif op.inc:
                        inst.then_inc(sem(("eng", engname)), 1)
            return body

        block.tensor(run("pe"))
        block.scalar(run("act"))
        block.vector(run("dve"))
        block.gpsimd(run("pool"))
        block.sync(run("sp"))


class B:
    def __init__(self, t, name):
        self.t = t
        self.r = Res(name)


def build_program(stage=99):
    nc = bass.Bass("TRN2", target_bir_lowering=False)
    nc.dge_precook = False
    S = Sched()
    st = ExitStack()

    def din(name, shape, dt=F32):
        return nc.dram_tensor(name, list(shape), dt, kind="ExternalInput").ap()

    def dout(name, shape):
        return nc.dram_tensor(name, list(shape), F32, kind="ExternalOutput").ap()

    xa = din("xa", [SEQ, D]); xo = din("xo", [2048, D]); xh = din("xh", [128, D]); hv = din("hv", [128, 4])
    xs = din("xs", [TS, D]); ck = din("ck", [PAST, 256]); cv = din("cv", [PAST, 256]); cki = din("cki", [PAST, 64])
    scvT = din("scvT", [D, 32], F32R); ccT = din("ccT", [128, 8, 2])
    w_ada = din("w_ada", [D, 6144], F32R); b_adaT = din("b_adaT", [128, 48])
    w_in = din("w_in", [D, N_IN], F32R); w_kv = din("w_kv", [D, 576], F32R); w_wi = din("w_wi", [D, 8], F32R)
    w_att = din("w_att", [D, D], F32R); w_co = din("w_co", [D, D], F32R); w_o = din("w_o", [D, D], F32R)
    wdwT = din("wdwT", [128, 8, 31]); cvec = din("cvec", [128, 3, 8])
    lnrow = din("lnrow", [4, D])
    w_rt = din("w_rt", [D, 20]); b_rt = din("b_rt", [1, 20])
    w_gu = din("w_gu", [16, D, 512], F32R); w_dn = din("w_dn", [16, 256, D], F32R)
    rope_all = din("rope_all", [SEQ, 96]); rope_own = din("rope_own", [2048, 96]); rope_s = din("rope_s", [TS, 96])
    amask = din("amask", [4, 128, 640]); ident_d = din("ident", [128, 128]); pw2 = din("pw2", [128, NIT])
    yo = dout("yo", [2048, D]); kvo = dout("kvo", [SEQ, 576]); cvo = dout("cvo", [32, D])
    ys = dout("ys", [TS, D]); kvs = dout("kvs", [TS, 576]); cvs = dout("cvs", [32, D])
    gscr = nc.dram_tensor("gscr", [32, 128], F32, kind="Internal").ap()

    def sb(name, shape, dt=F32):
        return st.enter_context(nc.sbuf_tensor(name, list(shape), dt))

    WS = [B(sb("WS%d" % i, [128, 8, 512], F32R), "WS%d" % i) for i in range(3)]
    HT = B(sb("HT", [128, 8, 544], F32R), "HT")
    HTR = {(g, p): Res("HT_%d_%d" % (g, p)) for g in range(5) for p in range(2)}

    def HTres(g):
        return [HTR[(g, 0)], HTR[(g, 1)]]

    HT_TOK = [HTR[(g, p)] for g in range(1, 5) for p in range(2)]
    A3 = B(sb("A3", [128, 8, 512], F32R), "A3")
    OX = B(sb("OX", [128, 8, 512], F32R), "OX")
    ysq = B(sb("ysq", [128, 512], F32R), "ysq")
    identr = B(sb("identr", [128, 128], F32R), "identr")
    onesr = B(sb("onesr", [128, 128], F32R), "onesr")
    siluT = B(sb("siluT", [128, 8, 2], F32R), "siluT")
    wwi = B(sb("wwi", [128, 8, 8], F32R), "wwi")
    KT = B(sb("KT", [128, 2, SEQ], BF16), "KT")
    VV = B(sb("VV", [128, 32, 256], BF16), "VV")
    KIT = B(sb("KIT", [128, SEQ], BF16), "KIT")
    A1 = B(sb("A1", [128, 4096], F32), "A1")
    A2 = B(sb("A2", [128, 4096], F32), "A2")
    A4 = B(sb("A4", [128, 3072], F32), "A4")
    XT = [B(sb("XT%d" % i, [128, D], F32), "XT%d" % i) for i in range(2)]
    kvsb = [B(sb("kvsb%d" % i, [128, 576], F32), "kvsb%d" % i) for i in range(2)]
    ident = B(sb("identf", [128, 128], F32), "identf")
    identb = B(sb("identb", [128, 128], BF16), "identb")
    onesb = B(sb("onesb", [128, 128], BF16), "onesb")
    modT = B(sb("modT", [128, 48, 2], F32), "modT")
    scp = B(sb("scp", [128, 2, 2, 8], F32), "scp")
    gpT = B(sb("gpT", [128, 32], F32), "gpT")
    badaT = B(sb("badaT", [128, 48], F32), "badaT")
    hvt = B(sb("hvt", [128, 4], F32), "hvt")
    wdw = B(sb("wdw", [128, 8, 31], F32), "wdw")
    cvc = B(sb("cvc", [128, 3, 8], F32), "cvc")
    wrt = B(sb("wrt", [128, 8, 20], F32), "wrt")
    brt = B(sb("brt", [128, 20], F32), "brt")
    amk = B(sb("amk", [128, 640], BF16), "amk")
    pw = B(sb("pw", [128, NIT], F32), "pw")
    rp = [B(sb("rp%d" % i, [128, 96], F32), "rp%d" % i) for i in range(2)]
    kb16 = B(amk.t[:, 0:384], "kb16")
    wsa = B(sb("wsa", [128, 4, 8], F32), "wsa")
    sm = B(sb("sm", [128, 32], F32), "sm")
    stp = B(sb("stp", [128, 3, NIT], F32), "stp")
    st6 = B(sb("st6", [128, 2, 6], F32), "st6")
    mv = B(sb("mv", [128, 2], F32), "mv")
    rt = B(sb("rt", [128, 96], F32), "rt")
    comb = B(sb("comb", [128, 4, 16], F32), "comb")
    gsb = B(A1.t[0:32, 128:256], "gsb"); gsb.r = A1.r
    ubr = B(sb("ubr", [128, 544], F32R), "ubr")
    ubf = ubr.t[:].bitcast(F32)
    dgc = [B(sb("dgc%d" % i, [128, 128], F32R), "dgc%d" % i) for i in range(4)]
    tA = B(A1.t[:, 3072:3328].rearrange("p (h d) -> p h d", h=8), "tA"); tA.r = A1.r
    tB = B(A1.t[:, 3328:3584].rearrange("p (h d) -> p h d", h=8), "tB"); tB.r = A1.r
    A3f = A3.t[:].rearrange("p a b -> p (a b)").bitcast(F32)
    A3r = A3.t[:].rearrange("p a b -> p (a b)")
    RR = [B(A3r[:, i * 512:(i + 1) * 512], "RR%d" % i) for i in range(4)]
    DG = B(A3r[:, 2048:3072].rearrange("p (h q) -> p h q", h=8), "DG")
    ACTT = B(A3r[:, 0:1024].rearrange("p (a b) -> p a b", a=2), "ACTT")
    _k0b = kvsb[0].t[:, 0:512].bitcast(BF16)
    PT = [B(_k0b[:, i * 512:(i + 1) * 512], "PT%d" % i) for i in range(2)]
    rec = B(kvsb[1].t[:, 0:512], "rec")
    A4b = A4.t[:].bitcast(BF16)
    QT = B(A4b[:, 0:4096].rearrange("p (h t) -> p h t", h=8), "QT")
    QST = B(A4b[:, 4096:6144].rearrange("p (a t) -> p a t", a=4), "QST")
    QSTH = B(XT[1].t[:].bitcast(BF16).rearrange("p (a t) -> p a t", a=4), "QSTH")
    ub = B(A4.t[:, 0:544], "ub")
    sg = B(A4.t[:, 544:1088], "sg")
    acc = B(A4.t[:, 1088:1600], "acc")
    A2b = A2.t[:].bitcast(BF16)
    MT = B(A2b[:, 0:4096].rearrange("p (k q) -> p k q", q=128), "MT")
    MQ = B(A2b[:, 4096:8192], "MQ")
    qb16 = B(A2b[:, 6144:8192], "qb16")
    lnt = B(A2.t[:, 0:1536].rearrange("p (a b) -> p a b", a=3), "lnt")
    uo = B(A2.t[0:32, 2048:3072], "uo")
    junk = B(XT[0].t[:].bitcast(mybir.dt.uint8), "junk"); junk.r = XT[0].r
    sgu = B(A1.t[:, 3072:3584], "sgu")

    def handoff(olds, news):
        for n_ in news:
            for o_ in olds:
                toks = ([o_.r.w] if o_.r.w is not None else []) + list(o_.r.r.values())
                for k_, t_ in enumerate(toks):
                    n_.r.r[("h", id(o_), k_)] = t_

    pst = st.enter_context(nc.psum_tensor("ps", [128, 7, 512], F32))
    psbf = st.enter_context(nc.psum_tensor("psbf", [128, 1024], BF16))
    PS = [B(None, "PS%d" % i) for i in range(8)]

    def ps(b, lo=0, hi=512):
        return pst[:, b, lo:hi]

    def psb(b):
        assert b == 7
        return psbf

    def A(eng, fn, reads=(), writes=()):
        return S.add(eng, fn, reads, writes)

    wcount = [0]

    def wload(src_ap, rows_kc=8, cols=512):
        slot = WS[wcount[0] % 3]
        wcount[0] += 1
        S.dma("sp", lambda e: e.dma_start(out=slot.t[:, 0:rows_kc, 0:cols],
                                          in_=src_ap.rearrange("(c p) n -> p c n", p=128)), writes=[slot])
        return slot

    out_toks = []

    S.dma("sp", lambda e: e.dma_start(out=ident.t[:], in_=ident_d), writes=[ident])
    A("dve", lambda e: e.tensor_copy(identb.t[:], ident.t[:]), [ident], [identb])
    A("act", lambda e: e.activation(out=identr.t[:], in_=ident.t[:], func=AF.Copy), [ident], [identr])
    A("dve", lambda e: e.memset(onesb.t[:], 1.0), [], [onesb])
    A("dve", lambda e: e.memset(A1.t[:, 0:128], 1.0), [], [A1])
    A("act", lambda e: e.activation(out=onesr.t[:], in_=A1.t[:, 0:128], func=AF.Copy), [A1], [onesr])
    for (dst, src) in [(badaT, b_adaT), (hvt, hv), (wdw, wdwT), (cvc, cvec), (pw, pw2)]:
        S.dma("sp", lambda e, dst=dst, src=src: e.dma_start(out=dst.t[:], in_=src), writes=[dst])
    S.dma("sp", lambda e: e.dma_start(out=wwi.t[:], in_=w_wi.rearrange("(c p) n -> p c n", p=128)), writes=[wwi])
    S.dma("sp", lambda e: e.dma_start(out=wrt.t[:], in_=w_rt.rearrange("(c p) n -> p c n", p=128)), writes=[wrt])
    S.dma("sp", lambda e: e.dma_start(out=brt.t[:], in_=b_rt.to_broadcast([128, 20])), writes=[brt])
    S.dma("sp", lambda e: e.dma_start(out=A2.t[:, 0:16].rearrange("p (a b) -> p a b", a=8), in_=ccT), writes=[A2])
    A("act", lambda e: e.activation(out=siluT.t[:], in_=A2.t[:, 0:16].rearrange("p (a b) -> p a b", a=8), func=AF.Silu), [A2], [siluT])

    for blk in range(12):
        slot = wload(w_ada[:, blk * 512:(blk + 1) * 512])
        for jj in range(4):
            j = blk * 4 + jj
            for kc in range(8):
                A("pe", lambda e, slot=slot, jj=jj, kc=kc, j=j: e.matmul(
                    pst[:, 0, 2 * j:2 * j + 2], slot.t[:, kc, jj * 128:(jj + 1) * 128], siluT.t[:, kc, :],
                    start=(kc == 0), stop=(kc == 7)), [slot, siluT], [PS[0]])
    A("dve", lambda e: e.tensor_tensor(out=modT.t[:], in0=pst[:, 0, 0:96].rearrange("p (j s) -> p j s", s=2),
                                       in1=badaT.t[:].unsqueeze(2).to_broadcast([128, 48, 2]), op=ALU.add),
      [PS[0], badaT], [modT])
    for v, base in enumerate((8, 32)):
        A("dve", lambda e, v=v, base=base: e.tensor_scalar(
            out=scp.t[:, v, :, :], in0=modT.t[:, base:base + 8, :].rearrange("p k s -> p s k"),
            scalar1=1.0, scalar2=None, op0=ALU.add), [modT], [scp])
    for v, base in enumerate((16, 40)):
        A("dve", lambda e, v=v, base=base: e.tensor_scalar(
            out=gpT.t[:, v * 16:(v + 1) * 16].rearrange("p (s k) -> p s k", s=2),
            in0=modT.t[:, base:base + 8, :].rearrange("p k s -> p s k"),
            scalar1=1.0, scalar2=None, op0=ALU.add), [modT], [gpT])
    A("pe", lambda e: e.transpose(pst[0:32, 1, 0:128], gpT.t[:], ident.t[:]), [gpT, ident], [PS[1]])
    A("dve", lambda e: e.tensor_copy(gsb.t[:], pst[0:32, 1, 0:128]), [PS[1]], [gsb])
    gs_res = Res("gscr")
    S.dma("pool", lambda e: e.dma_start(out=gscr, in_=gsb.t[:]), reads=[gsb], writes=[gs_res])
    gview = gscr.rearrange("(a k) p -> a (k p)", k=8)

    def finish():
        S.final_wait("pool", out_toks)
        S.emit(nc, st)
        st.close()
        return nc

    if stage == 0:
        out_toks.append(S.dma("pool", lambda e: e.dma_start(out=cvo[:, 0:128], in_=gsb.t[:]), reads=[gsb]))
        return finish()

    def sh_col(v, s, kc):
        base = 0 if v == 0 else 24
        return modT.t[:, base + kc, s:s + 1]

    def sc_col(v, s, kc):
        return scp.t[:, v, s, kc:kc + 1]

    xcnt = [0]

    def load_x(src_rows_ap, nrows=128):
        xb = XT[xcnt[0] % 2]
        xcnt[0] += 1
        S.dma("sp", lambda e: e.dma_start(out=xb.t[0:nrows, :], in_=src_rows_ap), writes=[xb])
        return xb

    tcnt = [0]

    def transpose_mod(xb, nrows, dst, dcol, v, s, src_t=None, g=None):
        pb = 2 + 2 * (tcnt[0] % 2)
        tcnt[0] += 1
        srct = xb.t if src_t is None else src_t
        for kc in range(8):
            b = pb + kc // 4
            A("pe", lambda e, kc=kc, b=b: e.transpose(pst[:, b, (kc % 4) * 128:(kc % 4) * 128 + nrows],
                                                      srct[0:nrows, kc * 128:(kc + 1) * 128], ident.t[0:nrows, 0:nrows]),
              [xb, ident], [PS[b]])
        for kc in range(8):
            b = pb + kc // 4
            src = pst[:, b, (kc % 4) * 128:(kc % 4) * 128 + nrows]
            if kc < 4:
                A("dve", lambda e, kc=kc, src=src: e.tensor_scalar(
                    out=dst.t[:, kc, dcol:dcol + nrows], in0=src, scalar1=sc_col(v, s, kc), scalar2=sh_col(v, s, kc),
                    op0=ALU.mult, op1=ALU.add), [PS[b], scp, modT], [HTR[(g, 0)]])
            else:
                A("act", lambda e, kc=kc, src=src: e.activation(
                    out=dst.t[:, kc, dcol:dcol + nrows], in_=src, func=AF.Identity,
                    scale=sc_col(v, s, kc), bias=sh_col(v, s, kc)), [PS[b], scp, modT], [HTR[(g, 1)]])

    def rope(buf, col0, nheads, hstride, half, tab, tcol, nrows):
        rot = 2 * half
        xv = buf.t[0:nrows, col0:col0 + nheads * hstride].rearrange("p (h d) -> p h d", h=nheads)
        cc = tab.t[0:nrows, tcol:tcol + rot].unsqueeze(1).to_broadcast([nrows, nheads, rot])
        sn1 = tab.t[0:nrows, tcol + rot:tcol + rot + half].unsqueeze(1).to_broadcast([nrows, nheads, half])
        sn2 = tab.t[0:nrows, tcol + rot + half:tcol + 2 * rot].unsqueeze(1).to_broadcast([nrows, nheads, half])
        a = tA.t[0:nrows, 0:nheads, 0:rot]
        b_ = tB.t[0:nrows, 0:nheads, 0:rot]
        A("dve", lambda e: e.tensor_tensor(out=a, in0=xv[:, :, 0:rot], in1=cc, op=ALU.mult), [buf, tab], [tA])
        A("dve", lambda e: e.tensor_tensor(out=b_[:, :, 0:half], in0=xv[:, :, half:rot], in1=sn1, op=ALU.mult), [buf, tab], [tB])
        A("dve", lambda e: e.tensor_tensor(out=b_[:, :, half:rot], in0=xv[:, :, 0:half], in1=sn2, op=ALU.mult), [buf, tab], [tB])
        A("dve", lambda e: e.tensor_tensor(out=xv[:, :, 0:rot], in0=a, in1=b_, op=ALU.add), [tA, tB], [buf])

    kvcnt = [0]

    import os
    SUB = int(os.environ.get("SUB", "9"))
    S.dma("sp", lambda e: e.dma_start(out=WS[0].t[:, :, 0:512], in_=w_kv[:, 0:512].rearrange("(c p) n -> p c n", p=128)), writes=[WS[0]])
    S.dma("sp", lambda e: e.dma_start(out=WS[1].t[:, :, 0:64], in_=w_kv[:, 512:576].rearrange("(c p) n -> p c n", p=128)), writes=[WS[1]])

    def kv_store(kvb, nrows, key_tile, krow0=0):
        A("act", lambda e: e.activation(out=VV.t[krow0:krow0 + nrows, key_tile, :], in_=kvb.t[0:nrows, 256:512], func=AF.Copy), [kvb], [VV])
        A("dve", lambda e: e.tensor_copy(kb16.t[0:nrows, 0:256], kvb.t[0:nrows, 0:256]), [kvb], [kb16])
        A("dve", lambda e: e.tensor_copy(kb16.t[0:nrows, 256:384].rearrange("p (s d) -> p s d", s=2),
                                         kvb.t[0:nrows, 512:576].unsqueeze(1).to_broadcast([nrows, 2, 64])), [kvb], [kb16])
        pv = psbf
        for g in range(2):
            A("pe", lambda e, g=g: e.transpose(pv[:, 256 + g * 128:256 + g * 128 + nrows], kb16.t[0:nrows, g * 128:(g + 1) * 128],
                                               identb.t[0:nrows, 0:nrows]), [kb16, identb], [PS[7]])
        A("pe", lambda e: e.transpose(pv[:, 512:512 + nrows], kb16.t[0:nrows, 256:384], identb.t[0:nrows, 0:nrows]),
          [kb16, identb], [PS[7]])
        k0 = key_tile * 128 + krow0
        A("act", lambda e: e.activation(out=KT.t[:, :, k0:k0 + nrows], in_=pv[:, 256:512].rearrange("p (g t) -> p g t", g=2)[:, :, 0:nrows],
                                        func=AF.Copy), [PS[7]], [KT])
        A("act", lambda e: e.activation(out=KIT.t[:, k0:k0 + nrows], in_=pv[:, 512:512 + nrows], func=AF.Copy), [PS[7]], [KIT])

    def kv_tile2(hT, hcol, nrows, rope_src_ap, out_ap, key_tile, store=True, g=1):
        i = kvcnt[0] % 2
        kvcnt[0] += 1
        kvb = kvsb[i]
        rpb = rp[i]
        pb = 0
        S.dma("sp", lambda e: e.dma_start(out=rpb.t[0:nrows, :], in_=rope_src_ap), writes=[rpb])
        for kc in range(8):
            A("pe", lambda e, kc=kc: e.matmul(pst[0:nrows, pb, 0:512], hT.t[:, kc, hcol:hcol + nrows], WS[0].t[:, kc, 0:512],
                                              start=(kc == 0), stop=(kc == 7)), HTres(g) + [WS[0]], [PS[pb]])
            A("pe", lambda e, kc=kc: e.matmul(pst[0:nrows, pb + 1, 0:64], hT.t[:, kc, hcol:hcol + nrows], WS[1].t[:, kc, 0:64],
                                              start=(kc == 0), stop=(kc == 7)), HTres(g) + [WS[1]], [PS[pb + 1]])
        A("act", lambda e: e.activation(out=kvb.t[0:nrows, 0:512], in_=pst[0:nrows, pb, 0:512], func=AF.Copy), [PS[pb]], [kvb])
        A("dve", lambda e: e.tensor_copy(kvb.t[0:nrows, 512:576], pst[0:nrows, pb + 1, 0:64]), [PS[pb + 1]], [kvb])
        rope(kvb, 0, 2, 128, 16, rpb, 0, nrows)
        rope(kvb, 512, 1, 64, 8, rpb, 64, nrows)
        out_toks.append(S.dma("pool", lambda e: e.dma_start(out=out_ap, in_=kvb.t[0:nrows, :]), reads=[kvb]))
        if store:
            kv_store(kvb, nrows, key_tile)
        return kvb

    def pp_stage1(n):
        xb = load_x(xa[n * 128:(n + 1) * 128, :])
        transpose_mod(xb, 128, HT, 32 + (n % 4) * 128, 0, 0, g=1 + n % 4)

    pp_kvb = {}
    pp_stage1(0)
    for n in range(32):
        if n + 1 < 32:
            pp_stage1(n + 1)
        pp_kvb[n] = kv_tile2(HT, 32 + (n % 4) * 128, 128, rope_all[n * 128:(n + 1) * 128, :], kvo[n * 128:(n + 1) * 128, :], n, store=False, g=1 + n % 4)
        if n >= 1:
            kv_store(pp_kvb[n - 1], 128, n - 1)
    kv_store(pp_kvb[31], 128, 31)

    if stage == 1:
        return finish()

    OXf = OX.t[:].rearrange("p a b -> p (a b)").bitcast(F32)
    OXr = OX.t[:].rearrange("p a b -> p (a b)")
    X1r = OXr.rearrange("p (t d) -> p t d", t=4)
    X1f = OXf.rearrange("p (t d) -> p t d", t=4)
    ZQ = [B(A1.t[:, 0:1536], "zq0"), B(A1.t[:, 1536:3072], "zq1"), B(A2.t[:, 0:1536], "zq2"), B(A2.t[:, 1536:3072], "zq3")]
    ZQ[0].r = A1.r; ZQ[1].r = A1.r; ZQ[2].r = A2.r; ZQ[3].r = A2.r
    SCR = B(A1.t, "SCR"); SCR.r = A1.r
    SG = B(A2.t[:, :].rearrange("p (c t) -> p c t", c=8), "SG"); SG.r = A2.r
    YA = B(A2.t[:, :].rearrange("p (t d) -> p t d", t=4), "YA"); YA.r = A2.r
    BC = [B(A1.t[:, i * 1024:(i + 1) * 1024], "BC%d" % i) for i in range(3)]
    BIG = 1.0e30

    def bn_ln(src_ap, nr, name):
        for h in range(2):
            A("dve", lambda e, h=h: e.bn_stats(out=st6.t[0:nr, h, :], in_=src_ap[:, h * 512:(h + 1) * 512]), [name], [st6])
        A("dve", lambda e: e.bn_aggr(out=mv.t[0:nr, :], in_=st6.t[0:nr].rearrange("p a b -> p (a b)")), [st6], [mv])
        A("act", lambda e: e.activation(out=sm.t[0:nr, 0:1], in_=mv.t[0:nr, 1:2], func=AF.Sqrt, bias=LN_EPS, scale=1.0), [mv], [sm])
        A("dve", lambda e: e.reciprocal(out=sm.t[0:nr, 0:1], in_=sm.t[0:nr, 0:1]), [sm], [sm])

    def make_hT_for(tiles, xsrc, halo_src, seq):
        if halo_src is not None:
            xb_ = load_x(halo_src, 32)
            transpose_mod(xb_, 32, HT, 0, 0, seq, g=0)
        for tt, (r0, nr) in enumerate(tiles):
            xb_ = load_x(xsrc[r0:r0 + nr, :], nr)
            transpose_mod(xb_, nr, HT, 32 + tt * 128, 0, seq, g=1 + tt)

    def process_st(ntok, tiles, xsrc, halo_src, halo_col, rope_src, seq, key_sizes_fn, use_amask, y_out, conv_out, conv_tok0,
                   hT_done=False, prefetch=None):
        nt = len(tiles)
        T0 = 32

        def make_hT():
            if halo_src is not None:
                xb_ = load_x(halo_src, 32)
                transpose_mod(xb_, 32, HT, 0, 0, seq, g=0)
            for tt, (r0, nr) in enumerate(tiles):
                xb_ = load_x(xsrc[r0:r0 + nr, :], nr)
                transpose_mod(xb_, nr, HT, T0 + tt * 128, 0, seq, g=1 + tt)

        if not hT_done:
            make_hT()
        handoff(kvsb, PT + [rec])
        for sl, c0 in enumerate((C_Q, C_Q + 512, C_QI)):
            slot = wload(w_in[:, c0:c0 + 512])
            for tt, (r0, nr) in enumerate(tiles):
                bank = (sl * nt + tt) % 2
                for kc in range(8):
                    A("pe", lambda e, kc=kc, bank=bank, tt=tt, nr=nr, slot=slot: e.matmul(
                        pst[0:nr, bank, :], HT.t[:, kc, T0 + tt * 128:T0 + tt * 128 + nr], slot.t[:, kc, :],
                        start=(kc == 0), stop=(kc == 7)), HTres(1 + tt) + [slot], [PS[bank]])
                if (sl + tt) % 2 == 0:
                    A("act", lambda e, bank=bank, tt=tt, nr=nr, sl=sl: e.activation(
                        out=ZQ[tt].t[0:nr, sl * 512:(sl + 1) * 512], in_=pst[0:nr, bank, :], func=AF.Copy), [PS[bank]], [ZQ[tt]])
                else:
                    A("dve", lambda e, bank=bank, tt=tt, nr=nr, sl=sl: e.tensor_copy(
                        ZQ[tt].t[0:nr, sl * 512:(sl + 1) * 512], pst[0:nr, bank, :]), [PS[bank]], [ZQ[tt]])
        for tt, (r0, nr) in enumerate(tiles):
            for kc in range(8):
                A("pe", lambda e, kc=kc, tt=tt, nr=nr: e.matmul(
                    pst[0:nr, 6, 0:8], HT.t[:, kc, T0 + tt * 128:T0 + tt * 128 + nr], wwi.t[:, kc, :],
                    start=(kc == 0), stop=(kc == 7)), HTres(1 + tt) + [wwi], [PS[6]])
            A("dve", lambda e, tt=tt, nr=nr: e.tensor_scalar(out=wsa.t[0:nr, tt, :], in0=pst[0:nr, 6, 0:8], scalar1=0.5 * ISCALE, scalar2=None,
                                                             op0=ALU.mult), [PS[6]], [wsa])
        for tt, (r0, nr) in enumerate(tiles):
            rpb = rp[tt % 2]
            S.dma("sp", lambda e, rpb=rpb, r0=r0, nr=nr: e.dma_start(out=rpb.t[0:nr, :], in_=rope_src[r0:r0 + nr, :]), writes=[rpb])
            rope(ZQ[tt], 0, 8, 128, 16, rpb, 0, nr)
            rope(ZQ[tt], 1024, 8, 64, 8, rpb, 64, nr)
            A("dve", lambda e, tt=tt, nr=nr: e.tensor_copy(qb16.t[0:nr, 0:1024], ZQ[tt].t[0:nr, 0:1024]), [ZQ[tt]], [qb16])
            A("dve", lambda e, tt=tt, nr=nr: e.tensor_copy(
                qb16.t[0:nr, 1024:2048].rearrange("p (h s d) -> p h s d", h=8, s=2),
                ZQ[tt].t[0:nr, 1024:1536].rearrange("p (h d) -> p h d", h=8).unsqueeze(2).to_broadcast([nr, 8, 2, 64])), [ZQ[tt]], [qb16])
            for h in range(8):
                A("pe", lambda e, h=h, nr=nr: e.transpose(psbf[:, h * 128:h * 128 + nr], qb16.t[0:nr, h * 128:(h + 1) * 128],
                                                          identb.t[0:nr, 0:nr]), [qb16, identb], [PS[7]])
            A("dve", lambda e, tt=tt, nr=nr: e.tensor_copy(
                QT.t[:, :, tt * 128:tt * 128 + nr], psbf[:, :].rearrange("p (h t) -> p h t", h=8)[:, :, 0:nr]), [PS[7]], [QT])
            for h in range(8):
                A("pe", lambda e, h=h, nr=nr: e.transpose(psbf[:, h * 128:h * 128 + nr], qb16.t[0:nr, 1024 + h * 128:1024 + (h + 1) * 128],
                                                          identb.t[0:nr, 0:nr]), [qb16, identb], [PS[7]])
            if tt == 0:
                handoff([XT[1]], [QSTH])
            A("dve", lambda e, tt=tt, nr=nr: e.tensor_copy(
                QST.t[:, :, tt * 128:tt * 128 + nr], psbf[:, 0:512].rearrange("p (a t) -> p a t", a=4)[:, :, 0:nr]), [PS[7]], [QST])
            A("dve", lambda e, tt=tt, nr=nr: e.tensor_copy(
                QSTH.t[:, :, tt * 128:tt * 128 + nr], psbf[:, 512:1024].rearrange("p (a t) -> p a t", a=4)[:, :, 0:nr]), [PS[7]], [QSTH])

        handoff([qb16, A2], [MQ, MT])
        if use_amask:
            S.dma("sp", lambda e: e.dma_start(out=A2.t[:, 3200:3840], in_=amask[halo_col]), writes=[MQ])
            handoff([kb16], [amk])
            A("dve", lambda e: e.tensor_copy(amk.t[:], A2.t[:, 3200:3840]), [MQ], [amk])

        def score_tile(t):
            r0, nq = tiles[t]
            ks = key_sizes_fn(t)
            L = sum(ks)
            q0 = t * 128
            for h in range(8):
                A("dve", lambda e, h=h, nq=nq, t=t: e.tensor_scalar(
                    out=DG.t[0:nq, h, 0:nq], in0=ident.t[0:nq, 0:nq], scalar1=wsa.t[0:nq, t, h:h + 1], scalar2=None, op0=ALU.mult),
                  [ident, wsa], [DG])
            items = []
            k0 = 0
            while k0 < L:
                kn = min(512, L - k0)
                for h in range(8):
                    items.append((k0, kn, h))
                k0 += kn
            n_it = len(items)

            DB = (0, 1, 3, 4)

            def dots(i):
                k0, kn, h = items[i]
                bank = DB[i % 4]
                qs_ = QST if h < 4 else QSTH
                A("pe", lambda e: e.matmul(
                    pst[0:nq, bank, 0:kn], qs_.t[:, h % 4, q0:q0 + nq], KIT.t[:, k0:k0 + kn], start=True, stop=True),
                  [qs_, KIT], [PS[bank]])
                rr = RR[i % 4]
                if i % 2 == 0:
                    A("act", lambda e: e.activation(out=rr.t[0:nq, 0:kn], in_=pst[0:nq, bank, 0:kn], func=AF.Relu), [PS[bank]], [rr])
                else:
                    A("dve", lambda e: e.tensor_scalar(out=rr.t[0:nq, 0:kn], in0=pst[0:nq, bank, 0:kn], scalar1=0.0, scalar2=None, op0=ALU.max),
                      [PS[bank]], [rr])

            def accm(i):
                k0, kn, h = items[i]
                rr = RR[i % 4]
                scb = (2, 5)[(i // 8) % 2]
                A("pe", lambda e: e.matmul(pst[0:nq, scb, 0:kn], DG.t[0:nq, h, 0:nq], rr.t[0:nq, 0:kn], start=(h == 0), stop=(h == 7)),
                  [DG, rr], [PS[scb]])
                if h == 7:
                    A("act", lambda e: e.activation(out=SCR.t[0:nq, k0:k0 + kn], in_=pst[0:nq, scb, 0:kn], func=AF.Copy), [PS[scb]], [SCR])

            for i in range(min(3, n_it)):
                dots(i)
            for i in range(n_it):
                if i + 3 < n_it:
                    dots(i + 3)
                accm(i)
            return L

        def thresh_tile(t, L):
            r0, nq = tiles[t]
            ks = key_sizes_fn(t)
            sc = SCR.t[0:nq, 0:L]
            c = lambda i: sm.t[0:nq, i:i + 1]
            A("dve", lambda e: e.tensor_reduce(out=c(8), in_=sc, op=ALU.max, axis=AX.X), [SCR], [sm])
            A("dve", lambda e: e.tensor_reduce(out=c(9), in_=sc, op=ALU.min, axis=AX.X), [SCR], [sm])
            A("dve", lambda e: e.tensor_tensor(out=c(10), in0=c(8), in1=c(9), op=ALU.subtract), [sm], [sm])
            A("dve", lambda e: e.tensor_scalar(out=c(10), in0=c(10), scalar1=0.505, scalar2=1e-6, op0=ALU.mult, op1=ALU.add), [sm], [sm])
            A("dve", lambda e: e.tensor_tensor(out=c(11), in0=c(8), in1=c(9), op=ALU.add), [sm], [sm])
            A("dve", lambda e: e.tensor_scalar(out=c(11), in0=c(11), scalar1=0.5, scalar2=None, op0=ALU.mult), [sm], [sm])
            A("dve", lambda e: e.tensor_scalar(out=stp.t[0:nq, 0, :], in0=pw.t[0:nq, :], scalar1=c(10), scalar2=None, op0=ALU.mult), [pw, sm], [stp])
            A("dve", lambda e: e.tensor_scalar(out=stp.t[0:nq, 1, :], in0=stp.t[0:nq, 0, :], scalar1=2.0, scalar2=None, op0=ALU.mult), [stp], [stp])
            A("dve", lambda e: e.tensor_scalar(out=stp.t[0:nq, 2, :], in0=stp.t[0:nq, 0, :], scalar1=-1.0, scalar2=None, op0=ALU.mult), [stp], [stp])
            if use_amask:
                A("dve", lambda e: e.tensor_tensor(out=SCR.t[0:nq, L - 640:L], in0=SCR.t[0:nq, L - 640:L], in1=amk.t[0:nq, :], op=ALU.add),
                  [SCR, amk], [SCR])
            for k in range(NIT):
                A("dve", lambda e: e.tensor_scalar(out=junk.t[0:nq, 0:L], in0=sc, scalar1=c(11), scalar2=None,
                                                   op0=ALU.is_ge, op1=ALU.add, accum_out=c(12)), [SCR, sm], [junk, sm])
                A("dve", lambda e, k=k: e.tensor_scalar(out=c(14), in0=c(12), scalar1=TOPK - 0.5, scalar2=stp.t[0:nq, 1, k:k + 1],
                                                        op0=ALU.is_ge, op1=ALU.mult), [sm, stp], [sm])
                A("dve", lambda e, k=k: e.tensor_scalar(out=c(11), in0=c(14), scalar1=c(11), scalar2=stp.t[0:nq, 2, k:k + 1],
                                                        op0=ALU.add, op1=ALU.add), [sm, stp], [sm])
            A("dve", lambda e: e.tensor_tensor(out=c(11), in0=c(11), in1=stp.t[0:nq, 2, NIT - 1:NIT], op=ALU.add), [sm, stp], [sm])
            A("dve", lambda e: e.tensor_scalar(out=MQ.t[0:nq, 0:L], in0=sc, scalar1=c(11), scalar2=MASKV, op0=ALU.is_lt, op1=ALU.mult),
              [SCR, sm], [MQ])
            nk = len(ks)
            kt = 0
            while kt < nk:
                nb_ = min(8, nk - kt)
                for j in range(nb_):
                    kk = kt + j
                    A("pe", lambda e, j=j, kk=kk: e.transpose(psbf[0:ks[kk], j * 128:j * 128 + nq], MQ.t[0:nq, kk * 128:kk * 128 + ks[kk]],
                                                              identb.t[0:nq, 0:nq]), [MQ, identb], [PS[7]])
                A("dve", lambda e, kt=kt, nb_=nb_: e.tensor_copy(
                    MT.t[:, kt:kt + nb_, 0:nq], psbf[:, 0:nb_ * 128].rearrange("p (k q) -> p k q", q=128)[:, :, 0:nq]), [PS[7]], [MT])
                kt += nb_

        def attend_tile(t):
            r0, nq = tiles[t]
            ks = key_sizes_fn(t)
            q0 = t * 128
            items = [(g, kk, kn) for g in range(2) for kk, kn in enumerate(ks)]
            n_it = len(items)

            def st_(i):
                g, kk, kn = items[i]
                sb_ = 3 + (i % 2)
                pt = PT[i % 2]
                A("pe", lambda e: e.matmul(
                    pst[0:kn, sb_, 0:4 * nq].rearrange("p (h q) -> p h q", h=4), KT.t[:, g, kk * 128:kk * 128 + kn],
                    QT.t[:, 4 * g:4 * g + 4, q0:q0 + nq], start=True, stop=False), [KT, QT], [PS[sb_]])
                A("pe", lambda e: e.matmul(
                    pst[0:kn, sb_, 0:4 * nq].rearrange("p (h q) -> p h q", h=4), identb.t[0:kn, 0:kn],
                    MT.t[0:kn, kk, 0:nq].unsqueeze(1).to_broadcast([kn, 4, nq]), start=False, stop=True), [identb, MT], [PS[sb_]])
                A("act", lambda e: e.activation(out=pt.t[0:kn, 0:4 * nq], in_=pst[0:kn, sb_, 0:4 * nq], func=AF.Exp, scale=ASCALE),
                  [PS[sb_]], [pt])

            def pv_(i):
                g, kk, kn = items[i]
                pt = PT[i % 2]
                ob, db = (5, 6) if (g == 0 or ATT_OLD) else (0, 1)
                A("pe", lambda e: e.matmul(pst[:, ob, 0:4 * nq], VV.t[0:kn, kk, g * 128:(g + 1) * 128], pt.t[0:kn, 0:4 * nq],
                                           start=(kk == 0), stop=(kk == len(ks) - 1)), [VV, pt], [PS[ob]])
                A("pe", lambda e: e.matmul(pst[:, db, 0:4 * nq], onesb.t[0:kn, :], pt.t[0:kn, 0:4 * nq],
                                           start=(kk == 0), stop=(kk == len(ks) - 1)), [onesb, pt], [PS[db]])

            st_(0)
            for i in range(n_it):
                if i + 1 < n_it:
                    st_(i + 1)
                pv_(i)
                if ATT_OLD and items[i][1] == len(ks) - 1:
                    attend_fin(t, [items[i][0]])

        def attend_fin(t, gs=(0, 1)):
            r0, nq = tiles[t]
            q0 = t * 128
            for g in gs:
                ob, db = (5, 6) if (g == 0 or ATT_OLD) else (0, 1)
                A("dve", lambda e, db=db: e.reciprocal(out=rec.t[:, 0:4 * nq], in_=pst[:, db, 0:4 * nq]), [PS[db]], [rec])
                A("dve", lambda e, g=g, ob=ob: e.tensor_tensor(
                    out=OX.t[:, 4 * g:4 * g + 4, q0:q0 + nq], in0=pst[:, ob, 0:4 * nq].rearrange("p (h q) -> p h q", h=4),
                    in1=rec.t[:, 0:4 * nq].rearrange("p (h q) -> p h q", h=4), op=ALU.mult), [PS[ob], rec], [OX])

        Ls = {0: score_tile(0)}
        thresh_tile(0, Ls[0])
        for t in range(nt):
            if t + 1 < nt:
                Ls[t + 1] = score_tile(t + 1)
            attend_tile(t)
            if t + 1 < nt:
                thresh_tile(t + 1, Ls[t + 1])
            if not ATT_OLD:
                attend_fin(t)

        handoff([QSTH], [XT[1]])
        make_hT()
        bankc = [0]

        def proj_fm(wsrc, c0, rhs_fn, rhsB, post):
            for hh in range(2):
                slot = wload(wsrc[:, c0 + hh * 512:c0 + (hh + 1) * 512])
                for jj in range(4):
                    j = hh * 4 + jj
                    bank = bankc[0] % 4
                    bankc[0] += 1
                    for kc in range(8):
                        A("pe", lambda e, kc=kc, slot=slot, bank=bank, jj=jj: e.matmul(
                            pst[:, bank, 0:ntok], slot.t[:, kc, jj * 128:(jj + 1) * 128], rhs_fn(kc), start=(kc == 0), stop=(kc == 7)),
                          [slot] + (rhsB if isinstance(rhsB, list) else [rhsB]), [PS[bank]])
                    post(j, bank)

        def post_sig(j, bank):
            A("act", lambda e, j=j, bank=bank: e.activation(out=SG.t[:, j, 0:ntok], in_=pst[:, bank, 0:ntok], func=AF.Sigmoid), [PS[bank]], [SG])

        def post_m1(j, bank):
            A("dve", lambda e, j=j, bank=bank: e.tensor_tensor(out=A3.t[:, j, 0:ntok], in0=pst[:, bank, 0:ntok], in1=SG.t[:, j, 0:ntok], op=ALU.mult),
              [PS[bank], SG], [A3])

        def post_m2(j, bank):
            A("dve", lambda e, j=j, bank=bank: e.tensor_tensor(out=acc.t[:, 0:ntok], in0=pst[:, bank, 0:ntok], in1=SG.t[:, j, 0:ntok], op=ALU.mult),
              [PS[bank], SG], [acc])
            A("dve", lambda e, j=j: e.tensor_tensor(out=A3.t[:, j, 0:ntok], in0=A3f[:, j * 512:j * 512 + ntok], in1=acc.t[:, 0:ntok], op=ALU.add),
              [A3, acc], [A3])

        handoff([MT, MQ], [SG])
        handoff(RR + [DG], [A3])
        proj_fm(w_in, C_GATT, lambda kc: HT.t[:, kc, T0:T0 + ntok], HT_TOK, post_sig)
        proj_fm(w_att, 0, lambda kc: OX.t[:, kc, 0:ntok], OX, post_m1)

        handoff([QT, QST], [ub, sg, acc])
        handoff([SG], [lnt, uo])
        for hh in range(2):
            slot_a = wload(w_in[:, C_GA + hh * 512:C_GA + (hh + 1) * 512])
            slot_b = wload(w_in[:, C_GB + hh * 512:C_GB + (hh + 1) * 512])
            for jj in range(4):
                j = hh * 4 + jj
                for (slot, bank, hc) in ((slot_a, 0, 0), (slot_b, 1, 32)):
                    for kc in range(8):
                        A("pe", lambda e, kc=kc, slot=slot, bank=bank, jj=jj: e.matmul(
                            pst[:, bank, 0:ntok], slot.t[:, kc, jj * 128:(jj + 1) * 128], HT.t[:, kc, T0:T0 + ntok],
                            start=(kc == 0), stop=(kc == 7)), [slot] + HT_TOK, [PS[bank]])
                    if halo_src is not None:
                        for kc in range(8):
                            A("pe", lambda e, kc=kc, slot=slot, hc=hc, jj=jj: e.matmul(
                                pst[:, 2, hc:hc + 32], slot.t[:, kc, jj * 128:(jj + 1) * 128], HT.t[:, kc, 0:32],
                                start=(kc == 0), stop=(kc == 7)), [slot] + HTres(0), [PS[2]])
                A("act", lambda e: e.activation(out=sg.t[:, T0:T0 + ntok], in_=pst[:, 1, 0:ntok], func=AF.Sigmoid), [PS[1]], [sg])
                A("dve", lambda e: e.tensor_tensor(out=ubr.t[:, T0:T0 + ntok], in0=pst[:, 0, 0:ntok], in1=sg.t[:, T0:T0 + ntok], op=ALU.mult),
                  [PS[0], sg], [ubr])
                if halo_src is not None:
                    A("act", lambda e: e.activation(out=sg.t[:, 0:32], in_=pst[:, 2, 32:64], func=AF.Sigmoid), [PS[2]], [sg])
                    A("dve", lambda e: e.scalar_tensor_tensor(out=ubr.t[:, 0:32], in0=pst[:, 2, 0:32], scalar=hvt.t[:, halo_col:halo_col + 1],
                                                              in1=sg.t[:, 0:32], op0=ALU.mult, op1=ALU.mult), [PS[2], sg, hvt], [ubr])
                else:
                    S.dma("sp", lambda e, j=j: e.dma_start(out=ubr.t[:, 0:32], in_=scvT[j * 128:(j + 1) * 128, :]), writes=[ubr])
                if conv_out is not None:
                    A("pe", lambda e, jj=jj: e.transpose(pst[0:32, 4, jj * 128:jj * 128 + 128],
                                                         ubf[:, T0 + conv_tok0:T0 + conv_tok0 + 32], ident.t[:, :]), [ubr, ident], [PS[4]])
                    A("act", lambda e, j=j, jj=jj: e.activation(out=uo.t[0:32, j * 128:(j + 1) * 128],
                                                                in_=pst[0:32, 4, jj * 128:jj * 128 + 128], func=AF.Copy), [PS[4]], [uo])
                NPE = 22
                for k in range(NPE):
                    if k % 2 == 0:
                        dg_ = dgc[(k // 2) % 2]
                        A("dve", lambda e, j=j, k=k, dg_=dg_: e.tensor_scalar(out=dg_.t[:, :], in0=ident.t[:, :], scalar1=wdw.t[:, j, k:k + 1],
                                                                              scalar2=None, op0=ALU.mult), [ident, wdw], [dg_])
                    else:
                        dg_ = dgc[2 + (k // 2) % 2]
                        A("act", lambda e, j=j, k=k, dg_=dg_: e.activation(out=dg_.t[:, :], in_=ident.t[:, :], func=AF.Identity,
                                                                           scale=wdw.t[:, j, k:k + 1]), [ident, wdw], [dg_])
                    A("pe", lambda e, k=k, dg_=dg_: e.matmul(pst[:, 3, 0:ntok], dg_.t[:, :], ubr.t[:, 2 + k:2 + k + ntok],
                                                             start=(k == 0), stop=(k == NPE - 1)), [dg_, ubr], [PS[3]])
                    w = NPE + k
                    if w < 31:
                        if k == 0:
                            A("dve", lambda e, j=j, w=w: e.tensor_scalar(out=acc.t[:, 0:ntok], in0=ubf[:, 2 + w:2 + w + ntok], scalar1=wdw.t[:, j, w:w + 1],
                                                                         scalar2=cvc.t[:, 0, j:j + 1], op0=ALU.mult, op1=ALU.add), [ubr, wdw, cvc], [acc])
                        else:
                            A("dve", lambda e, j=j, w=w: e.scalar_tensor_tensor(
                                out=acc.t[:, 0:ntok], in0=ubf[:, 2 + w:2 + w + ntok], scalar=wdw.t[:, j, w:w + 1], in1=acc.t[:, 0:ntok],
                                op0=ALU.mult, op1=ALU.add), [ubr, wdw, acc], [acc])
                A("dve", lambda e, j=j: e.tensor_tensor(out=OX.t[:, j, 0:ntok], in0=pst[:, 3, 0:ntok], in1=acc.t[:, 0:ntok], op=ALU.add),
                  [PS[3], acc], [OX])
                A("act", lambda e, j=j: e.activation(out=ysq.t[:, 0:ntok], in_=OXf[:, j * 512:j * 512 + ntok], func=AF.Square), [OX], [ysq])
                A("pe", lambda e, j=j: e.matmul(pst[:, 5, 0:ntok], onesr.t[:, :], OX.t[:, j, 0:ntok], start=(j == 0), stop=(j == 7)),
                  [onesr, OX], [PS[5]])
                A("pe", lambda e, j=j: e.matmul(pst[:, 6, 0:ntok], onesr.t[:, :], ysq.t[:, 0:ntok], start=(j == 0), stop=(j == 7)),
                  [onesr, ysq], [PS[6]])
        if conv_out is not None:
            out_toks.append(S.dma("pool", lambda e: e.dma_start(out=conv_out, in_=uo.t[0:32, :]), reads=[uo]))
        mean_ = lnt.t[:, 0, 0:ntok]; rstd_ = lnt.t[:, 1, 0:ntok]; tmp_ = lnt.t[:, 2, 0:ntok]
        A("dve", lambda e: e.tensor_scalar(out=mean_, in0=pst[:, 5, 0:ntok], scalar1=1.0 / D, scalar2=None, op0=ALU.mult), [PS[5]], [lnt])
        A("dve", lambda e: e.tensor_tensor(out=tmp_, in0=mean_, in1=mean_, op=ALU.mult), [lnt], [lnt])
        A("dve", lambda e: e.scalar_tensor_tensor(out=rstd_, in0=pst[:, 6, 0:ntok], scalar=1.0 / D, in1=tmp_, op0=ALU.mult, op1=ALU.subtract),
          [PS[6], lnt], [lnt])
        A("act", lambda e: e.activation(out=rstd_, in_=rstd_, func=AF.Sqrt, bias=LN_EPS, scale=1.0), [lnt], [lnt])
        A("dve", lambda e: e.reciprocal(out=rstd_, in_=rstd_), [lnt], [lnt])
        for j in range(8):
            A("dve", lambda e, j=j: e.tensor_tensor(out=tmp_, in0=OXf[:, j * 512:j * 512 + ntok], in1=mean_, op=ALU.subtract), [OX, lnt], [lnt])
            A("dve", lambda e: e.tensor_tensor(out=tmp_, in0=tmp_, in1=rstd_, op=ALU.mult), [lnt], [lnt])
            A("act", lambda e, j=j: e.activation(out=OX.t[:, j, 0:ntok], in_=tmp_, func=AF.Silu, scale=cvc.t[:, 1, j:j + 1],
                                                 bias=cvc.t[:, 2, j:j + 1]), [lnt, cvc], [OX])

        handoff([lnt, uo], [SG])
        proj_fm(w_in, C_GCONV, lambda kc: HT.t[:, kc, T0:T0 + ntok], HT_TOK, post_sig)
        proj_fm(w_co, 0, lambda kc: OX.t[:, kc, 0:ntok], OX, post_m2)

        handoff([SCR], BC)
        S.dma("sp", lambda e: e.dma_start(out=BC[0].t, in_=gview[0 * 2 + seq:0 * 2 + seq + 1, :].to_broadcast([128, D])), reads=[gs_res], writes=[BC[0]])
        S.dma("sp", lambda e: e.dma_start(out=BC[1].t, in_=lnrow[0:1, :].to_broadcast([128, D])), writes=[BC[1]])
        S.dma("sp", lambda e: e.dma_start(out=BC[2].t, in_=lnrow[1:2, :].to_broadcast([128, D])), writes=[BC[2]])
        wo_slots = [wload(w_o[:, dh * 512:(dh + 1) * 512]) for dh in range(2)]

        def wo_tile(tt):
            r0, nr = tiles[tt]
            for dh in range(2):
                slot = wo_slots[dh]
                bank = dh
                for kc in range(8):
                    A("pe", lambda e, kc=kc, slot=slot, bank=bank: e.matmul(
                        pst[0:nr, bank, :], A3.t[:, kc, tt * 128:tt * 128 + nr], slot.t[:, kc, :], start=(kc == 0), stop=(kc == 7)),
                      [A3, slot], [PS[bank]])
                A("dve", lambda e, bank=bank, dh=dh: e.tensor_tensor(
                    out=X1r[0:nr, tt, dh * 512:(dh + 1) * 512], in0=pst[0:nr, bank, :], in1=BC[0].t[0:nr, dh * 512:(dh + 1) * 512], op=ALU.mult),
                  [PS[bank], BC[0]], [OX])

        def ln_tile(tt):
            r0, nr = tiles[tt]
            xb_ = load_x(xsrc[r0:r0 + nr, :], nr)
            x1w = X1r[0:nr, tt, :]
            x1rd = X1f[0:nr, tt, :]
            A("dve", lambda e: e.scalar_tensor_tensor(
                out=x1w, in0=xb_.t[0:nr, :], scalar=ALPHA, in1=x1rd, op0=ALU.mult, op1=ALU.add), [xb_, OX], [OX])
            bn_ln(x1rd, nr, OX)
            A("dve", lambda e: e.tensor_scalar(out=x1w, in0=x1rd, scalar1=mv.t[0:nr, 0:1], scalar2=sm.t[0:nr, 0:1],
                                               op0=ALU.subtract, op1=ALU.mult), [OX, mv, sm], [OX])
            A("dve", lambda e: e.tensor_tensor(out=x1w, in0=x1rd, in1=BC[1].t[0:nr, :], op=ALU.mult), [OX, BC[1]], [OX])
            A("dve", lambda e: e.tensor_tensor(out=x1w, in0=x1rd, in1=BC[2].t[0:nr, :], op=ALU.add), [OX, BC[2]], [OX])
            transpose_mod(OX, nr, HT, T0 + tt * 128, 1, seq, src_t=X1f[:, tt, :], g=1 + tt)

        wo_tile(0)
        for tt in range(nt):
            if tt + 1 < nt:
                wo_tile(tt + 1)
            ln_tile(tt)

        handoff(BC, [sgu])
        handoff([SG], [YA])
        for tt, (r0, nr) in enumerate(tiles):
            for kc in range(8):
                A("pe", lambda e, kc=kc, tt=tt, nr=nr: e.matmul(
                    pst[0:nr, 6, 0:20], HT.t[:, kc, T0 + tt * 128:T0 + tt * 128 + nr].bitcast(F32), wrt.t[:, kc, :],
                    start=(kc == 0), stop=(kc == 7)), HTres(1 + tt) + [wrt], [PS[6]])
            R = lambda a_, b_, nr=nr: rt.t[0:nr, a_:b_]
            A("dve", lambda e, nr=nr, R=R: e.tensor_tensor(out=R(0, 20), in0=pst[0:nr, 6, 0:20], in1=brt.t[0:nr, :], op=ALU.add), [PS[6], brt], [rt])
            A("dve", lambda e, R=R: e.tensor_reduce(out=R(20, 21), in_=R(0, 4), op=ALU.max, axis=AX.X), [rt], [rt])
            A("dve", lambda e, R=R: e.tensor_scalar(out=R(24, 28), in0=R(0, 4), scalar1=R(20, 21), scalar2=None, op0=ALU.subtract), [rt], [rt])
            A("act", lambda e, R=R: e.activation(out=R(28, 32), in_=R(24, 28), func=AF.Exp, accum_out=R(21, 22)), [rt], [rt])
            A("dve", lambda e, R=R: e.reciprocal(out=R(21, 22), in_=R(21, 22)), [rt], [rt])
            A("dve", lambda e, R=R: e.tensor_scalar(out=R(24, 28), in0=R(0, 4), scalar1=R(20, 21), scalar2=BIG, op0=ALU.is_ge, op1=ALU.mult), [rt], [rt])
            A("dve", lambda e, R=R: e.tensor_scalar(out=R(24, 28), in0=R(24, 28), scalar1=-BIG, scalar2=None, op0=ALU.add), [rt], [rt])
            A("dve", lambda e, R=R, nr=nr: e.tensor_tensor(
                out=R(32, 48).rearrange("p (g k) -> p g k", g=4), in0=R(4, 20).rearrange("p (g k) -> p g k", g=4),
                in1=R(24, 28).unsqueeze(2).to_broadcast([nr, 4, 4]), op=ALU.add), [rt], [rt])
            A("dve", lambda e, R=R: e.tensor_reduce(out=R(22, 23), in_=R(32, 48), op=ALU.max, axis=AX.X), [rt], [rt])
            A("dve", lambda e, R=R: e.tensor_scalar(out=R(48, 64), in0=R(32, 48), scalar1=R(22, 23), scalar2=None, op0=ALU.is_ge), [rt], [rt])
            A("dve", lambda e, R=R: e.scalar_tensor_tensor(out=R(64, 80), in0=R(48, 64), scalar=-BIG, in1=R(32, 48), op0=ALU.mult, op1=ALU.add), [rt], [rt])
            A("dve", lambda e, R=R: e.tensor_reduce(out=R(23, 24), in_=R(64, 80), op=ALU.max, axis=AX.X), [rt], [rt])
            A("dve", lambda e, R=R: e.tensor_scalar(out=R(80, 96), in0=R(64, 80), scalar1=R(23, 24), scalar2=None, op0=ALU.is_ge), [rt], [rt])
            A("dve", lambda e, R=R: e.tensor_tensor(out=R(24, 25), in0=R(23, 24), in1=R(22, 23), op=ALU.subtract), [rt], [rt])
            A("act", lambda e, R=R: e.activation(out=R(25, 26), in_=R(24, 25), func=AF.Exp), [rt], [rt])
            A("dve", lambda e, R=R: e.tensor_scalar(out=R(26, 27), in0=R(25, 26), scalar1=1.0, scalar2=None, op0=ALU.add), [rt], [rt])
            A("dve", lambda e, R=R: e.reciprocal(out=R(26, 27), in_=R(26, 27)), [rt], [rt])
            A("dve", lambda e, R=R: e.tensor_tensor(out=R(27, 28), in0=R(25, 26), in1=R(26, 27), op=ALU.mult), [rt], [rt])
            A("dve", lambda e, R=R: e.tensor_scalar(out=R(26, 28), in0=R(26, 28), scalar1=R(21, 22), scalar2=None, op0=ALU.mult), [rt], [rt])
            A("dve", lambda e, R=R: e.tensor_scalar(out=R(32, 48), in0=R(48, 64), scalar1=R(26, 27), scalar2=None, op0=ALU.mult), [rt], [rt])
            A("dve", lambda e, R=R, nr=nr, tt=tt: e.scalar_tensor_tensor(out=comb.t[0:nr, tt, :], in0=R(80, 96), scalar=R(27, 28),
                                                                         in1=R(32, 48), op0=ALU.mult, op1=ALU.add), [rt], [comb])
        dcnt = [0]
        ACT2 = [B(A3r[:, i * 1024:(i + 1) * 1024].rearrange("p (a b) -> p a b", a=2), "ACT2_%d" % i) for i in range(2)]
        SGU2 = [B(A1.t[:, 3072 + i * 512:3584 + i * 512], "sgu%d" % i) for i in range(2)]
        for o_ in ACT2:
            o_.r = Res(o_.r.name)
        handoff([A3], ACT2)
        handoff(BC + [sgu], SGU2)
        dn_views = {}

        def gu_(ex):
            slot_gu = WS[ex % 2]
            S.dma("sp", lambda e: e.dma_start(out=slot_gu.t[:, :, :], in_=w_gu[ex].rearrange("(c p) n -> p c n", p=128)), writes=[slot_gu])
            slot_dn = WSH[ex % 2]
            hk = 4 * (ex % 2)
            dnv = WS[2].t[:, hk:hk + 4, :].rearrange("p a b -> p (a b)").rearrange("p (f n) -> p f n", f=2)
            S.dma("sp", lambda e: e.dma_start(out=dnv, in_=w_dn[ex].rearrange("(f p) n -> p f n", p=128)), writes=[slot_dn])
            dn_views[ex] = (slot_dn, dnv)
            at = ACT2[ex % 2]
            for fc in range(2):
                for (c0, bank) in ((fc * 128, 0 + fc), (256 + fc * 128, 2 + fc)):
                    for kc in range(8):
                        A("pe", lambda e, kc=kc, c0=c0, bank=bank: e.matmul(
                            pst[:, bank, 0:ntok], slot_gu.t[:, kc, c0:c0 + 128], HT.t[:, kc, T0:T0 + ntok], start=(kc == 0), stop=(kc == 7)),
                          [slot_gu] + HT_TOK, [PS[bank]])
                su = SGU2[fc]
                A("act", lambda e, fc=fc, su=su: e.activation(out=su.t[:, 0:ntok], in_=pst[:, 0 + fc, 0:ntok], func=AF.Silu), [PS[0 + fc]], [su])
                A("dve", lambda e, fc=fc, su=su: e.tensor_tensor(out=at.t[:, fc, 0:ntok], in0=pst[:, 2 + fc, 0:ntok], in1=su.t[:, 0:ntok], op=ALU.mult),
                  [PS[2 + fc], su], [at])

        def down_(ex):
            slot_dn, dnv = dn_views[ex]
            at = ACT2[ex % 2]
            for tt, (r0, nr) in enumerate(tiles):
                for dh in range(2):
                    bank = 4 + dcnt[0] % 3
                    dcnt[0] += 1
                    for fc in range(2):
                        A("pe", lambda e, fc=fc, tt=tt, nr=nr, dh=dh, bank=bank: e.matmul(
                            pst[0:nr, bank, :], at.t[:, fc, tt * 128:tt * 128 + nr], dnv[:, fc, dh * 512:(dh + 1) * 512],
                            start=(fc == 0), stop=(fc == 1)), [at, slot_dn], [PS[bank]])
                    ya = YA.t[0:nr, tt, dh * 512:(dh + 1) * 512]
                    if ex == 0:
                        A("dve", lambda e, nr=nr, tt=tt, bank=bank, ya=ya: e.tensor_scalar(
                            out=ya, in0=pst[0:nr, bank, :], scalar1=comb.t[0:nr, tt, ex:ex + 1], scalar2=None, op0=ALU.mult), [PS[bank], comb], [YA])
                    else:
                        A("dve", lambda e, nr=nr, tt=tt, bank=bank, ya=ya: e.scalar_tensor_tensor(
                            out=ya, in0=pst[0:nr, bank, :], scalar=comb.t[0:nr, tt, ex:ex + 1], in1=ya, op0=ALU.mult, op1=ALU.add),
                          [PS[bank], comb, YA], [YA])

        WSH = [B(None, "WSH0"), B(None, "WSH1")]
        handoff([WS[2]], WSH)
        gu_(0)
        for ex in range(16):
            if ex + 1 < 16:
                gu_(ex + 1)
            down_(ex)
        handoff(WSH, [WS[2]])
        handoff(ACT2, [A3])
        handoff(SGU2, [sgu])
        if prefetch is not None:
            prefetch()
        handoff([sgu], BC)
        S.dma("sp", lambda e: e.dma_start(out=BC[0].t, in_=gview[1 * 2 + seq:1 * 2 + seq + 1, :].to_broadcast([128, D])), reads=[gs_res], writes=[BC[0]])
        S.dma("sp", lambda e: e.dma_start(out=BC[1].t, in_=lnrow[2:3, :].to_broadcast([128, D])), writes=[BC[1]])
        S.dma("sp", lambda e: e.dma_start(out=BC[2].t, in_=lnrow[3:4, :].to_broadcast([128, D])), writes=[BC[2]])
        for tt, (r0, nr) in enumerate(tiles):
            ya = YA.t[0:nr, tt, :]
            A("dve", lambda e, nr=nr, ya=ya: e.tensor_tensor(out=ya, in0=ya, in1=BC[0].t[0:nr, :], op=ALU.mult), [YA, BC[0]], [YA])
            A("dve", lambda e, nr=nr, tt=tt, ya=ya: e.scalar_tensor_tensor(out=ya, in0=X1f[0:nr, tt, :], scalar=ALPHA, in1=ya, op0=ALU.mult, op1=ALU.add),
              [OX, YA], [YA])
            bn_ln(ya, nr, YA)
            A("dve", lambda e, nr=nr, ya=ya: e.tensor_scalar(out=ya, in0=ya, scalar1=mv.t[0:nr, 0:1], scalar2=sm.t[0:nr, 0:1],
                                                             op0=ALU.subtract, op1=ALU.mult), [YA, mv, sm], [YA])
            A("dve", lambda e, nr=nr, ya=ya: e.tensor_tensor(out=ya, in0=ya, in1=BC[1].t[0:nr, :], op=ALU.mult), [YA, BC[1]], [YA])
            A("dve", lambda e, nr=nr, ya=ya: e.tensor_tensor(out=ya, in0=ya, in1=BC[2].t[0:nr, :], op=ALU.add), [YA, BC[2]], [YA])
            out_toks.append(S.dma("pool", lambda e, r0=r0, nr=nr, ya=ya: e.dma_start(out=y_out[r0:r0 + nr, :], in_=ya), reads=[YA]))
        handoff(BC, [A1])
        handoff([ub, sg, acc], [QT])
        handoff(PT + [rec], kvsb)
        handoff([YA], [qb16])
        handoff([A3], RR + [DG])

    NSTP = int(os.environ.get("NSTP", "4"))
    ATT_OLD = int(os.environ.get("ATT_OLD", "0"))
    def cache_prep():
        handoff(PT + [rec], kvsb)
        handoff([amk], [kb16])
        for n in range(8):
            kvb = kvsb[n % 2]
            S.dma("sp", lambda e, n=n, kvb=kvb: e.dma_start(out=kvb.t[:, 0:256], in_=ck[n * 128:(n + 1) * 128, :]), writes=[kvb])
            S.dma("sp", lambda e, n=n, kvb=kvb: e.dma_start(out=kvb.t[:, 256:512], in_=cv[n * 128:(n + 1) * 128, :]), writes=[kvb])
            S.dma("sp", lambda e, n=n, kvb=kvb: e.dma_start(out=kvb.t[:, 512:576], in_=cki[n * 128:(n + 1) * 128, :]), writes=[kvb])
            kv_store(kvb, 128, n)

    st_tiles = lambda i: [(i * 512 + t * 128, 128) for t in range(4)]
    for i in range(NSTP):
        nxt = cache_prep if (i == 3 and stage > 2) else None
        if i + 1 < NSTP:
            nxt = (lambda i=i: make_hT_for(st_tiles(i + 1), xo, xh[(i + 1) * 32:(i + 2) * 32, :], 0))
        process_st(512, st_tiles(i), xo, xh[i * 32:(i + 1) * 32, :], i, rope_own, 0,
                   (lambda t, i=i: [128] * (8 * i + 5 + t)), True, yo,
                   (cvo if i == 3 else None), 480, hT_done=(i > 0), prefetch=nxt)

    if stage == 2:
        return finish()

    S.dma("sp", lambda e: e.dma_start(out=WS[0].t[:, :, 0:512], in_=w_kv[:, 0:512].rearrange("(c p) n -> p c n", p=128)), writes=[WS[0]])
    S.dma("sp", lambda e: e.dma_start(out=WS[1].t[:, :, 0:64], in_=w_kv[:, 512:576].rearrange("(c p) n -> p c n", p=128)), writes=[WS[1]])
    wcount[0] = 2
    xb = load_x(xs, TS)
    transpose_mod(xb, TS, HT, 32, 0, 1, g=1)
    kv_tile2(HT, 32, TS, rope_s, kvs, 8, g=1)
    process_st(TS, [(0, TS)], xs, None, 0, rope_s, 1, (lambda t: [128] * 8 + [TS]), False, ys, cvs, 32)
    return finish()


def _rope_tab(pos):
    pos = np.asarray(pos, dtype=np.float32)
    out = np.zeros((pos.shape[0], 96), np.float32)
    for (half, c0) in ((16, 0), (8, 64)):
        inv = (np.float32(ROPE_THETA) ** (-np.arange(half, dtype=np.float32) / np.float32(half))).astype(np.float32)
        ang = (pos[:, None] * inv[None, :]).astype(np.float32)
        c = np.cos(ang).astype(np.float32)
        s = np.sin(ang).astype(np.float32)
        out[:, c0:c0 + half] = c
        out[:, c0 + half:c0 + 2 * half] = c
        out[:, c0 + 2 * half:c0 + 3 * half] = -s
        out[:, c0 + 3 * half:c0 + 4 * half] = s
    return out


OWN = ((0, 3, 4, 7), (1, 2, 5, 6))
_NC_CACHE = {}


def _colT(v):
    return np.ascontiguousarray(np.asarray(v, np.float32).reshape(8, 128).T)


def kernel(x_prompt, x_sample, cache_k, cache_v, cache_idx_k, state_conv, c_prompt, c_sample,
           w_ada, b_ada, w_in, w_att_out, w_dw, b_dw, conv_ln_g, conv_ln_b, w_conv_out, w_o,
           ln1_g, ln1_b, w_group, b_group, w_router, b_router, w_gate_up, w_down, ln2_g, ln2_b, _stage=99):
    f = lambda a: np.ascontiguousarray(np.asarray(a, dtype=np.float32))
    x_prompt = f(x_prompt); x_sample = f(x_sample)
    w_in0 = f(w_in)[0]
    common = {
        "w_ada": f(w_ada)[0], "b_adaT": np.ascontiguousarray(f(b_ada)[0].reshape(48, 128).T),
        "w_in": w_in0, "w_kv": np.ascontiguousarray(np.concatenate([w_in0[:, C_K:C_K + 512], w_in0[:, C_KI:C_KI + 64]], axis=1)), "w_wi": np.ascontiguousarray(w_in0[:, C_WI:C_WI + 8]),
        "w_att": f(w_att_out)[0], "w_co": f(w_conv_out)[0], "w_o": f(w_o)[0],
        "wdwT": np.ascontiguousarray(f(w_dw)[0].reshape(31, 8, 128).transpose(2, 1, 0)),
        "cvec": np.ascontiguousarray(np.stack([_colT(f(b_dw)[0]), _colT(f(conv_ln_g)[0]), _colT(f(conv_ln_b)[0])], axis=1)),
        "lnrow": np.ascontiguousarray(np.stack([f(ln1_g)[0], f(ln1_b)[0], f(ln2_g)[0], f(ln2_b)[0]], axis=0)),
        "w_rt": np.ascontiguousarray(np.concatenate([f(w_group)[0], f(w_router)[0]], axis=1)),
        "b_rt": np.ascontiguousarray(np.concatenate([f(b_group)[0], f(b_router)[0]])[None, :]),
        "w_gu": f(w_gate_up)[0], "w_dn": f(w_down)[0],
        "rope_all": _rope_tab(np.arange(SEQ)), "rope_s": _rope_tab(PAST + np.arange(TS)),
        "ident": np.eye(128, dtype=np.float32),
        "pw2": np.ascontiguousarray(np.tile((0.5 ** (np.arange(NIT) + 1)).astype(np.float32)[None, :], (128, 1))),
    }
    in_maps = []
    for c in range(8):
        b, hf = c // 2, c % 2
        own = OWN[hf]
        xb = x_prompt[b]
        xo = np.concatenate([xb[s * 512:(s + 1) * 512] for s in own], axis=0)
        xh = np.zeros((4, 32, D), np.float32)
        hv = np.zeros((128, 4), np.float32)
        for i, s in enumerate(own):
            if s > 0:
                xh[i] = xb[s * 512 - 32:s * 512]
                hv[:, i] = 1.0
        pos_own = np.concatenate([np.arange(s * 512, (s + 1) * 512) for s in own])
        am = np.zeros((4, 128, 5, 128), np.float32)
        diag = np.zeros((128, 128), np.float32)
        diag[0:64, 64:128] = NEG
        for i, s in enumerate(own):
            if s % 2 == 1:
                am[i, :, 4, :] = diag
            else:
                am[i, :, 0, :] = diag
                am[i, :, 1:, :] = NEG
        scv = np.zeros((32, D), np.float32)
        scv[2:] = f(state_conv)[0, c]
        ccT = np.stack([f(c_prompt)[b], f(c_sample)[c]], axis=0)
        ccT = np.ascontiguousarray(ccT.reshape(2, 8, 128).transpose(2, 1, 0))
        m = dict(common)
        m.update({
            "xa": xb, "xo": np.ascontiguousarray(xo), "xh": np.ascontiguousarray(xh.reshape(128, D)), "hv": hv,
            "xs": x_sample[c], "ck": np.ascontiguousarray(f(cache_k)[0, c].reshape(PAST, 256)),
            "cv": np.ascontiguousarray(f(cache_v)[0, c].reshape(PAST, 256)), "cki": f(cache_idx_k)[0, c],
            "scvT": np.ascontiguousarray(scv.T), "ccT": ccT,
            "rope_own": _rope_tab(pos_own), "amask": np.ascontiguousarray(am.reshape(4, 128, 640)),
        })
        in_maps.append(m)
    if _stage not in _NC_CACHE:
        _NC_CACHE[_stage] = build_program(_stage)
    nc = _NC_CACHE[_stage]
    res = run_bass_kernel_spmd(nc, in_maps, core_ids=list(range(8)))
    R = res.results
    yp = np.zeros((4, SEQ, D), np.float32)
    kp = np.zeros((1, 4, SEQ, 2, 128), np.float32)
    vp = np.zeros((1, 4, SEQ, 2, 128), np.float32)
    kip = np.zeros((1, 4, SEQ, 64), np.float32)
    cp = np.zeros((1, 4, 30, D), np.float32)
    ysa = np.zeros((8, TS, D), np.float32)
    ksa = np.zeros((1, 8, TS, 2, 128), np.float32)
    vsa = np.zeros((1, 8, TS, 2, 128), np.float32)
    kisa = np.zeros((1, 8, TS, 64), np.float32)
    csa = np.zeros((1, 8, 30, D), np.float32)
    for c in range(8):
        b, hf = c // 2, c % 2
        r = R[c]
        for i, s in enumerate(OWN[hf]):
            yp[b, s * 512:(s + 1) * 512] = r["yo"][i * 512:(i + 1) * 512]
            kv = r["kvo"][s * 512:(s + 1) * 512]
            kp[0, b, s * 512:(s + 1) * 512] = kv[:, 0:256].reshape(512, 2, 128)
            vp[0, b, s * 512:(s + 1) * 512] = kv[:, 256:512].reshape(512, 2, 128)
            kip[0, b, s * 512:(s + 1) * 512] = kv[:, 512:576]
        if hf == 0:
            cp[0, b] = r["cvo"][2:32]
        ysa[c] = r["ys"]
        ksa[0, c] = r["kvs"][:, 0:256].reshape(TS, 2, 128)
        vsa[0, c] = r["kvs"][:, 256:512].reshape(TS, 2, 128)
        kisa[0, c] = r["kvs"][:, 512:576]
        csa[0, c] = r["cvs"][2:32]
    return (yp, ysa, kp, vp, kip, cp, ksa, vsa, kisa, csa)
```
